# Optimizing a Trainium2 kernel written in Bass

```python
import math
import jax, jax.numpy as jnp
from jax import lax
import numpy as np

D_MODEL = 1024
BATCH = 2
SEQ = 8192
DEPTH = 1

D_MIX = D_MODEL
D_POOL = D_MIX // 4
POOL_WINDOWS = (2, 4, 8, 16)
N_POOL_GROUPS = len(POOL_WINDOWS)
POOL_GROUP = D_POOL // N_POOL_GROUPS
D_RWKV = D_MIX - D_POOL
HEAD_SIZE = 64
N_RWKV_HEADS = D_RWKV // HEAD_SIZE
D_DECAY_LORA = 64
D_AAA_LORA = 64
D_GATE_LORA = 128
D_RWKV_IN = 3 * D_RWKV + D_DECAY_LORA + D_AAA_LORA + D_GATE_LORA
D_IN = D_POOL + D_RWKV_IN

N_GROUPS = 4
EXPERTS_PER_GROUP = 8
N_EXPERTS = N_GROUPS * EXPERTS_PER_GROUP
TOP_K = 2
D_EXPERT = D_MODEL // 4
ROW_BLOCK = 128

LN_EPS = 1e-5
LNX_EPS = 64e-5
DEEPNORM_ALPHA = (2.0 * DEPTH) ** 0.25
DEEPNORM_BETA = (8.0 * DEPTH) ** -0.25

kernel_name = "hybrid_pool_rwkv7_hiermoe_deepnorm"


def layer_norm(x, g, b):
    xf = x.astype(jnp.float32)
    mu = jnp.mean(xf, axis=-1, keepdims=True)
    var = jnp.mean(jnp.square(xf - mu), axis=-1, keepdims=True)
    return ((xf - mu) * lax.rsqrt(var + LN_EPS) * g.astype(jnp.float32) + b.astype(jnp.float32)).astype(x.dtype)


def multiscale_pool(p, pool_w, pool_scale):
    b, s, _ = p.shape
    pf = p.astype(jnp.float32)
    csum = jnp.cumsum(pf, axis=1)
    count = jnp.arange(1, s + 1, dtype=jnp.float32)[None, :, None]
    outs = []
    for gi, win in enumerate(POOL_WINDOWS):
        c = csum[..., gi * POOL_GROUP:(gi + 1) * POOL_GROUP]
        c_lag = jnp.pad(c, ((0, 0), (win, 0), (0, 0)))[:, :s]
        mean = (c - c_lag) / jnp.minimum(count, float(win))
        outs.append(mean - pf[..., gi * POOL_GROUP:(gi + 1) * POOL_GROUP])
    diff = jnp.stack(outs, axis=2)
    mixed = jnp.einsum('bsgc,gcd->bsgd', diff, pool_w.astype(jnp.float32)).reshape(b, s, D_POOL)
    return (mixed * pool_scale.astype(jnp.float32)).astype(p.dtype)


def rwkv7_step(state, inp):
    r, decay, k, v, kk, a = inp
    sa = jnp.einsum('bhij,bhj->bhi', state, -kk)
    state = (state * decay[:, :, None, :]
             + sa[..., None] * (kk * a)[:, :, None, :]
             + v[..., None] * k[:, :, None, :])
    out = jnp.einsum('bhij,bhj->bhi', state, r)
    return state, out


def rwkv7_mix(z, mu_shift, w0, w_up, a0, a_up, g_up, k_k, k_a, r_k, lnx_g, lnx_b):
    b, s, _ = z.shape
    f32 = jnp.float32
    zf = z.astype(f32)
    prev = jnp.pad(zf, ((0, 0), (1, 0), (0, 0)))[:, :s]
    zm = zf + (prev - zf) * mu_shift.astype(f32)
    o = 0
    r = zm[..., o:o + D_RWKV]; o += D_RWKV
    k = zm[..., o:o + D_RWKV]; o += D_RWKV
    v = zm[..., o:o + D_RWKV]; o += D_RWKV
    wd = zm[..., o:o + D_DECAY_LORA]; o += D_DECAY_LORA
    ad = zm[..., o:o + D_AAA_LORA]; o += D_AAA_LORA
    gd = zm[..., o:o + D_GATE_LORA]
    w_log = -jax.nn.softplus(-(w0.astype(f32) + jnp.tanh(wd) @ w_up.astype(f32))) - 0.5
    decay = jnp.exp(-jnp.exp(w_log))
    a = jax.nn.sigmoid(a0.astype(f32) + ad @ a_up.astype(f32))
    g = jax.nn.sigmoid(gd) @ g_up.astype(f32)
    kk = k * k_k.astype(f32)
    k = k * (1.0 + (a - 1.0) * k_a.astype(f32))
    hs = lambda t: t.reshape(b, s, N_RWKV_HEADS, HEAD_SIZE)
    r, k, v, kk, a, decay = hs(r), hs(k), hs(v), hs(kk), hs(a), hs(decay)
    kk = kk / jnp.maximum(jnp.sqrt(jnp.sum(kk * kk, axis=-1, keepdims=True)), 1e-12)
    tmaj = lambda t: jnp.moveaxis(t, 1, 0)
    state0 = jnp.zeros((b, N_RWKV_HEADS, HEAD_SIZE, HEAD_SIZE), f32)
    _, y = lax.scan(rwkv7_step, state0, (tmaj(r), tmaj(decay), tmaj(k), tmaj(v), tmaj(kk), tmaj(a)))
    y = jnp.moveaxis(y, 0, 1)
    mu = jnp.mean(y, axis=-1, keepdims=True)
    var = jnp.mean(jnp.square(y - mu), axis=-1, keepdims=True)
    y = ((y - mu) * lax.rsqrt(var + LNX_EPS)).reshape(b, s, D_RWKV) * lnx_g.astype(f32) + lnx_b.astype(f32)
    bonus = jnp.sum(r * k * r_k.astype(f32), axis=-1, keepdims=True) * v
    y = (y + bonus.reshape(b, s, D_RWKV)) * g
    return y.astype(z.dtype)


def hierarchical_moe(h, router_group, router_group_b, router_expert, router_expert_b,
                     exp_gate, exp_up, exp_down):
    n, d = h.shape
    hf = h.astype(jnp.float32)
    g_prob = jax.nn.softmax(hf @ router_group.astype(jnp.float32) + router_group_b.astype(jnp.float32), axis=-1)
    g_top_p, g_idx = lax.top_k(g_prob, 1)
    e_logits = (hf @ router_expert.astype(jnp.float32) + router_expert_b.astype(jnp.float32)
                ).reshape(n, N_GROUPS, EXPERTS_PER_GROUP)
    e_sel = jnp.take_along_axis(e_logits, g_idx[:, :, None], axis=1)[:, 0]
    e_top_logit, e_idx = lax.top_k(e_sel, TOP_K)
    weights = g_top_p * jax.nn.softmax(e_top_logit, axis=-1)
    expert_id = g_idx * EXPERTS_PER_GROUP + e_idx

    m = n * TOP_K
    flat_e = expert_id.reshape(m).astype(jnp.int32)
    flat_tok = jnp.repeat(jnp.arange(n, dtype=jnp.int32), TOP_K)
    flat_w = weights.reshape(m)
    order = jnp.argsort(flat_e)
    e_sorted, tok_sorted, w_sorted = flat_e[order], flat_tok[order], flat_w[order]
    counts = jnp.bincount(flat_e, length=N_EXPERTS)
    padded = (counts + ROW_BLOCK - 1) // ROW_BLOCK * ROW_BLOCK
    start = jnp.cumsum(counts) - counts
    pend = jnp.cumsum(padded)
    pstart = pend - padded
    dest = pstart[e_sorted] + (jnp.arange(m, dtype=jnp.int32) - start[e_sorted])
    m_pad = (m + ROW_BLOCK - 1) // ROW_BLOCK * ROW_BLOCK + N_EXPERTS * ROW_BLOCK
    n_blocks = m_pad // ROW_BLOCK
    row_tok = jnp.zeros((m_pad,), jnp.int32).at[dest].set(tok_sorted)
    row_w = jnp.zeros((m_pad,), h.dtype).at[dest].set(w_sorted.astype(h.dtype))
    block_start = jnp.arange(n_blocks, dtype=jnp.int32) * ROW_BLOCK
    block_expert = jnp.minimum(jnp.searchsorted(pend, block_start, side='right'), N_EXPERTS - 1)

    def expert_block(args):
        tok, wgt, e = args
        xb = h[tok]
        hid = jax.nn.silu(xb @ exp_gate[e]) * (xb @ exp_up[e])
        return (hid @ exp_down[e]) * wgt[:, None]

    ys = lax.map(expert_block, (row_tok.reshape(n_blocks, ROW_BLOCK),
                                row_w.reshape(n_blocks, ROW_BLOCK), block_expert))
    return jnp.zeros((n, d), h.dtype).at[row_tok].add(ys.reshape(m_pad, d))


def hybrid_layer(x, w_in, pool_w, pool_scale, mu_shift, w0, w_up, a0, a_up, g_up, k_k, k_a, r_k,
                 lnx_g, lnx_b, w_out, ln1_g, ln1_b, router_group, router_group_b, router_expert,
                 router_expert_b, exp_gate, exp_up, exp_down, ln2_g, ln2_b):
    b, s, d = x.shape
    proj = x @ w_in
    pool_out = multiscale_pool(proj[..., :D_POOL], pool_w, pool_scale)
    rwkv_out = rwkv7_mix(proj[..., D_POOL:], mu_shift, w0, w_up, a0, a_up, g_up,
                         k_k, k_a, r_k, lnx_g, lnx_b)
    mixed = jnp.concatenate([pool_out, rwkv_out], axis=-1) @ w_out
    h = layer_norm(DEEPNORM_ALPHA * x + mixed, ln1_g, ln1_b)
    ffn = hierarchical_moe(h.reshape(b * s, d), router_group, router_group_b, router_expert,
                           router_expert_b, exp_gate, exp_up, exp_down).reshape(b, s, d)
    return layer_norm(DEEPNORM_ALPHA * h + ffn, ln2_g, ln2_b)


def setup_inputs(seed: int = 0) -> dict:
    key = jax.random.key(seed)
    ks = jax.random.split(key, 28)
    nrm = lambda k, shape, scale: jax.random.normal(k, shape, jnp.float32) * scale
    L = DEPTH
    return {
        "x": nrm(ks[0], (BATCH, SEQ, D_MODEL), 1.0),
        "w_in": nrm(ks[1], (L, D_MODEL, D_IN), D_MODEL ** -0.5),
        "pool_w": nrm(ks[2], (L, N_POOL_GROUPS, POOL_GROUP, POOL_GROUP), POOL_GROUP ** -0.5),
        "pool_scale": 1.0 + nrm(ks[3], (L, D_POOL), 0.1),
        "mu_shift": jax.random.uniform(ks[4], (L, D_RWKV_IN), jnp.float32),
        "w0": jnp.linspace(-6.0, -1.0, D_RWKV, dtype=jnp.float32)[None, :] + nrm(ks[5], (L, D_RWKV), 0.1),
        "w_up": nrm(ks[6], (L, D_DECAY_LORA, D_RWKV), 0.1),
        "a0": nrm(ks[7], (L, D_RWKV), 0.1),
        "a_up": nrm(ks[8], (L, D_AAA_LORA, D_RWKV), 0.1),
        "g_up": nrm(ks[9], (L, D_GATE_LORA, D_RWKV), D_GATE_LORA ** -0.5),
        "k_k": 0.85 + nrm(ks[10], (L, D_RWKV), 0.05),
        "k_a": 1.0 + nrm(ks[11], (L, D_RWKV), 0.05),
        "r_k": nrm(ks[12], (L, N_RWKV_HEADS, HEAD_SIZE), 0.1),
        "lnx_g": 1.0 + nrm(ks[13], (L, D_RWKV), 0.1),
        "lnx_b": nrm(ks[14], (L, D_RWKV), 0.01),
        "w_out": nrm(ks[15], (L, D_MIX, D_MODEL), D_MIX ** -0.5 * DEEPNORM_BETA),
        "ln1_g": 1.0 + nrm(ks[16], (L, D_MODEL), 0.05),
        "ln1_b": nrm(ks[17], (L, D_MODEL), 0.01),
        "router_group": nrm(ks[18], (L, D_MODEL, N_GROUPS), D_MODEL ** -0.5),
        "router_group_b": nrm(ks[19], (L, N_GROUPS), 0.01),
        "router_expert": nrm(ks[20], (L, D_MODEL, N_EXPERTS), D_MODEL ** -0.5),
        "router_expert_b": nrm(ks[21], (L, N_EXPERTS), 0.01),
        "exp_gate": nrm(ks[22], (L, N_EXPERTS, D_MODEL, D_EXPERT), D_MODEL ** -0.5),
        "exp_up": nrm(ks[23], (L, N_EXPERTS, D_MODEL, D_EXPERT), D_MODEL ** -0.5),
        "exp_down": nrm(ks[24], (L, N_EXPERTS, D_EXPERT, D_MODEL), D_EXPERT ** -0.5 * DEEPNORM_BETA),
        "ln2_g": 1.0 + nrm(ks[25], (L, D_MODEL), 0.05),
        "ln2_b": nrm(ks[26], (L, D_MODEL), 0.01),
    }


def reference(x, w_in, pool_w, pool_scale, mu_shift, w0, w_up, a0, a_up, g_up, k_k, k_a, r_k,
              lnx_g, lnx_b, w_out, ln1_g, ln1_b, router_group, router_group_b, router_expert,
              router_expert_b, exp_gate, exp_up, exp_down, ln2_g, ln2_b):
    for l in range(DEPTH):
        x = hybrid_layer(x, w_in[l], pool_w[l], pool_scale[l], mu_shift[l], w0[l], w_up[l], a0[l],
                         a_up[l], g_up[l], k_k[l], k_a[l], r_k[l], lnx_g[l], lnx_b[l], w_out[l],
                         ln1_g[l], ln1_b[l], router_group[l], router_group_b[l], router_expert[l],
                         router_expert_b[l], exp_gate[l], exp_up[l], exp_down[l], ln2_g[l], ln2_b[l])
    return x
```

```python
import numpy as np
import concourse.bass as bass
import concourse.mybir as mybir
from concourse.bass_utils import run_bass_kernel_spmd

F32 = mybir.dt.float32
BF16 = mybir.dt.bfloat16
ALU = mybir.AluOpType
AF = mybir.ActivationFunctionType

D = 1024
SEQ = 8192
NCORE = 8
OWN = 2048
TT = 256
NTILE = SEQ // TT
OWN0 = (SEQ - OWN) // TT
NCH = TT // 128
DIN = 2816
ALPHA = 2.0 ** 0.25
SC = -float(np.exp(-0.5))
LN_EPS = 1e-5
LNX_EPS = 64e-5
NDMASEM = 6

PP_MU, PP_W0, PP_A0, PP_KK, PP_KA, PP_PS, PP_N = 0, 20, 26, 32, 38, 44, 48
BC_LNXG, BC_LNXB, BC_LN1G, BC_LN1B, BC_N = 0, 768, 1536, 2560, 3584
BC2_LN2G, BC2_LN2B, BC2_RB, BC2_N = 0, 1024, 2048, 2084


class Prog:
    def __init__(self, nc):
        self.nc = nc
        self.engs = {"pe": nc.tensor, "act": nc.scalar, "dve": nc.vector, "pool": nc.gpsimd, "sp": nc.sync}
        self.q = {e: [] for e in self.engs}
        self.cnt = {e: 0 for e in self.engs}
        self.seen = {e: {} for e in self.engs}
        self.lastw = {}
        self.readers = {}
        self.dma_val = {}
        self.dma_rr = {"sp": 0, "pool": 0}
        self.log = []

    def _deps(self, reads, writes, eng=None):
        deps = []
        for k in reads:
            if k in self.lastw:
                deps.append(self.lastw[k])
        for k in writes:
            isbank = isinstance(k, tuple) and k[0] == "B"
            if k in self.lastw:
                d = self.lastw[k]
                if not (isbank and d[0] == eng):
                    deps.append(d)
            for d in self.readers.get(k, ()):
                if not (isbank and d[0] == eng):
                    deps.append(d)
        return deps

    def _waits(self, eng, deps):
        need = {}
        for (s, v) in deps:
            if s == eng and eng == "pe":
                continue
            if need.get(s, 0) < v:
                need[s] = v
        out = []
        for s, v in need.items():
            if self.seen[eng].get(s, 0) >= v:
                continue
            self.seen[eng][s] = v
            out.append((s, v))
        return out

    def _record(self, me, reads, writes):
        for k in reads:
            self.readers.setdefault(k, []).append(me)
        for k in writes:
            self.lastw[k] = me
            self.readers[k] = []

    def op(self, eng, fn, reads=(), writes=()):
        waits = self._waits(eng, self._deps(reads, writes, eng))
        self.cnt[eng] += 1
        me = (eng, self.cnt[eng])
        self.q[eng].append((waits, fn, (eng, 1)))
        self._record(me, reads, writes)
        self.log.append((eng, me, waits, list(reads), list(writes)))
        return me

    def dma(self, eng, fn, reads=(), writes=()):
        i = self.dma_rr[eng]
        self.dma_rr[eng] = (i + 1) % NDMASEM
        s = "dma_%s_%d" % (eng, i)
        prev = self.dma_val.get(s, 0)
        deps = self._deps(reads, writes)
        if prev:
            deps.append((s, prev))
        waits = self._waits(eng, deps)
        self.dma_val[s] = prev + 16
        me = (s, prev + 16)
        self.q[eng].append((waits, fn, (s, 16)))
        self._record(me, reads, writes)
        self.log.append((eng + "-dma", me, waits, list(reads), list(writes)))
        return me

    def barrier(self):
        allsig = [(e, c) for e, c in self.cnt.items() if c > 0 and e != "sp"]
        allsig += [(s, v) for s, v in self.dma_val.items()]
        for eng in self.engs:
            waits = self._waits(eng, [d for d in allsig if d[0] != eng])
            if waits:
                self.q[eng].append((waits, None, None))
        self.lastw = {}
        self.readers = {}

    def sem_names(self):
        names = ["pe", "act", "dve", "pool"]
        for eng in ("sp", "pool"):
            for i in range(NDMASEM):
                names.append("dma_%s_%d" % (eng, i))
        return names

    def emit(self, sems):
        nc = self.nc
        with nc.Block() as block:
            def mk(engname):
                def body(e):
                    for waits, fn, inc in self.q[engname]:
                        for s, v in waits:
                            e.wait_ge(sems[s], v)
                        if fn is not None:
                            ins = fn(e)
                            ins.then_inc(sems[inc[0]], inc[1])
                return body
            block.tensor(mk("pe"))
            block.scalar(mk("act"))
            block.vector(mk("dve"))
            block.gpsimd(mk("pool"))
            block.sync(mk("sp"))


class Arena:
    def __init__(self, ap, cap):
        self.ap = ap
        self.cap = cap
        self.off = 0
        self.peak = 0

    def f32(self, n):
        n2 = (n + 1) // 2 * 2
        o = self.off
        self.off += n2
        self.peak = max(self.peak, self.off)
        assert self.off <= self.cap, ("arena overflow", self.off, self.cap)
        return self.ap[:, o:o + n]

    def bf(self, n):
        u = (n + 1) // 2
        u = (u + 1) // 2 * 2
        o = self.off
        self.off += u
        self.peak = max(self.peak, self.off)
        assert self.off <= self.cap, ("arena overflow", self.off, self.cap)
        return self.ap[:, o:o + u].bitcast(BF16)[:, 0:n]


class StopBuild(Exception):
    pass


def build(start_tile=0, do_moe=True, debug_h=False, stop_at=None):
    nc = bass.Bass("TRN2", target_bir_lowering=False)
    P = Prog(nc)

    def din(name, shape):
        return nc.dram_tensor(name, list(shape), F32, kind="ExternalInput").ap()

    x_ext = din("x_ext", [SEQ, D])
    pos0_d = din("pos0", [128, 1])
    w_in_d = din("w_in", [D, DIN])
    pp_d = din("pp", [128, PP_N])
    bc_d = din("bc", [128, BC_N])
    bc2_d = din("bc2", [128, BC2_N])
    rk_d = din("rkm", [128, 12])
    poolbd_d = din("poolbd", [256, 128])
    loraup_d = din("loraup", [128, 768])
    gup_d = din("gup", [128, 768])
    w_out_d = din("w_out", [D, D])
    router_d = din("router", [D, 36])
    eg_d = din("exp_gate", [32, D, 256])
    eu_d = din("exp_up", [32, D, 256])
    ed_d = din("exp_down", [32, 256, D])
    out_d = nc.dram_tensor("out", [OWN, D], F32, kind="ExternalOutput").ap()
    hbuf = nc.dram_tensor("hbuf", [OWN, D], F32).ap()

    CAP = 45000
    arena_t = nc.sbuf_tensor("arena", [128, CAP], F32)
    psum_t = nc.psum_tensor("ps", [128, 4096], F32)
    arena_h = arena_t.__enter__()
    psum_h = psum_t.__enter__()
    A = Arena(arena_h[:, :], CAP)
    ps = psum_h

    def bank(b):
        return ps[:, b * 512:(b + 1) * 512]

    def TTo(eng, out, in0, in1, op, r, w):
        P.op(eng, lambda e: e.tensor_tensor(out=out, in0=in0, in1=in1, op=op), r, w)

    def TS(eng, out, in0, s1, s2, op0, op1, r, w):
        if op1 is None:
            P.op(eng, lambda e: e.tensor_scalar(out=out, in0=in0, scalar1=s1, scalar2=None, op0=op0), r, w)
        else:
            P.op(eng, lambda e: e.tensor_scalar(out=out, in0=in0, scalar1=s1, scalar2=s2, op0=op0, op1=op1), r, w)

    def STT(out, in0, scalar, in1, op0, op1, r, w):
        P.op("dve", lambda e: e.scalar_tensor_tensor(out=out, in0=in0, scalar=scalar, in1=in1, op0=op0, op1=op1), r, w)

    def ACT(out, in_, func, r, w, bias=None, scale=None):
        kw = {}
        if bias is not None:
            kw["bias"] = bias
        if scale is not None:
            kw["scale"] = scale
        P.op("act", lambda e: e.activation(out=out, in_=in_, func=func, **kw), r, w)

    def CP(eng, out, in_, r, w):
        if eng == "act":
            ACT(out, in_, AF.Copy, r, w)
        else:
            P.op(eng, lambda e: e.tensor_copy(out=out, in_=in_), r, w)

    def MM(out, lhsT, rhs, start, stop, r, w):
        P.op("pe", lambda e: e.matmul(out, lhsT, rhs, start=start, stop=stop), r, w)

    def DMA(eng, out, in_, r, w):
        P.dma(eng, lambda e: e.dma_start(out=out, in_=in_), r, w)


    def chk(name, items):
        if stop_at != name:
            return
        for i, (ap, key) in enumerate(items):
            n = ap.shape[-1]
            scr = dbgscr[:, 0:n]
            CP("pool", scr, ap, [key], ["dbgscr"])
            DMA("sp", out_d[i * 128:(i + 1) * 128, 0:n], scr, ["dbgscr"], [("dbgout", i)])
        raise StopBuild()

    dbgscr = A.f32(1024) if stop_at else None
    ident = A.bf(128)
    MU = A.bf(512)
    SL = A.bf(128)
    bones = A.bf(128)
    onesf = A.f32(128)
    zeros = A.f32(128)
    pp = A.f32(PP_N)
    omk = A.f32(6)
    bc = A.f32(BC_N)
    rkf = A.f32(12)
    RK = A.bf(12)
    poolbd = A.bf(256).rearrange("p (q d) -> p q d", q=2)
    loraup = A.bf(768)
    gup = A.bf(768)
    w_in = A.bf(8 * DIN).rearrange("p (k n) -> p k n", k=8)
    w_out = A.bf(8 * D).rearrange("p (k n) -> p k n", k=8)
    pos0 = A.f32(2)
    carry = A.f32(24)
    Hf = A.f32(6 * 64).rearrange("p (a b) -> p a b", a=6)
    Hb = A.bf(6 * 64).rearrange("p (a b) -> p a b", a=6)
    tmpH = A.f32(64)
    epsx = A.f32(2)
    eps1 = A.f32(2)

    P.op("pool", lambda e: e.memset(onesf, 1.0), [], ["onesf"])
    P.op("pool", lambda e: e.memset(zeros, 0.0), [], ["zeros"])
    P.op("pool", lambda e: e.memset(carry, 0.0), [], ["carry"])
    P.op("pool", lambda e: e.memset(Hf.rearrange("p a b -> p (a b)"), 0.0), [], ["Hf"])
    P.op("pool", lambda e: e.memset(Hb.rearrange("p a b -> p (a b)"), 0.0), [], ["Hb"])
    P.op("pool", lambda e: e.memset(epsx, LNX_EPS), [], ["epsx"])
    P.op("pool", lambda e: e.memset(eps1, LN_EPS), [], ["eps1"])
    P.op("pool", lambda e: e.memset(bones, 0.0), [], ["bones"])
    P.op("pool", lambda e: e.memset(bones[0:64, 0:64], 1.0), [], ["bones"])
    P.op("pool", lambda e: e.memset(bones[64:128, 64:128], 1.0), [], ["bones"])
    P.op("pool", lambda e: e.affine_select(out=ident, in_=onesf, pattern=[[-1, 128]], compare_op=ALU.is_equal,
                                           fill=0.0, base=0, channel_multiplier=1), ["onesf"], ["ident"])
    P.op("pool", lambda e: e.affine_select(out=MU[:, 0:128], in_=onesf, pattern=[[1, 128]], compare_op=ALU.is_gt,
                                           fill=0.0, base=0, channel_multiplier=-1), ["onesf"], ["MU"])
    P.op("pool", lambda e: e.affine_select(out=MU[:, 128:256], in_=onesf, pattern=[[1, 128]], compare_op=ALU.is_ge,
                                           fill=0.0, base=0, channel_multiplier=-1), ["onesf"], ["MU"])
    P.op("pool", lambda e: e.tensor_copy(out=MU[:, 256:512], in_=MU[:, 0:256]), ["MU"], ["MU"])
    P.op("pool", lambda e: e.affine_select(out=SL, in_=onesf, pattern=[[-1, 128]], compare_op=ALU.is_gt,
                                           fill=0.0, base=0, channel_multiplier=1), ["onesf"], ["SL"])
    DMA("sp", pp, pp_d, [], ["pp"])
    DMA("sp", bc, bc_d, [], ["bc"])
    DMA("sp", rkf, rk_d, [], ["rkf"])
    DMA("sp", pos0[:, 0:1], pos0_d, [], ["pos0"])
    CP("pool", RK, rkf, ["rkf"], ["RK"])
    TS("pool", omk, pp[:, PP_KA:PP_KA + 6], -1.0, 1.0, ALU.mult, ALU.add, ["pp"], ["omk"])
    DMA("pool", poolbd, poolbd_d.rearrange("(q p) d -> p q d", p=128), [], ["poolbd"])
    DMA("pool", loraup, loraup_d, [], ["loraup"])
    DMA("pool", gup, gup_d, [], ["gup"])
    w_in_v = w_in_d.rearrange("(k p) n -> p k n", p=128)
    for kc in range(8):
        for hh in range(2):
            DMA("pool", w_in[:, kc, hh * 1408:(hh + 1) * 1408], w_in_v[:, kc, hh * 1408:(hh + 1) * 1408], [], ["w_in"])
    w_out_v = w_out_d.rearrange("(k p) n -> p k n", p=128)
    for kc in range(8):
        DMA("pool", w_out[:, kc, :], w_out_v[:, kc, :], [], ["w_out"])

    xs = [A.bf(NCH * D).rearrange("p (n d) -> p n d", n=NCH) for _ in range(2)]
    xT = A.bf(8 * TT).rearrange("p (k t) -> p k t", k=8)
    lor = A.bf(TT)
    sgb = A.bf(TT)
    zs = [A.f32(TT + 2) for _ in range(2)]
    dtmp = [A.f32(TT) for _ in range(2)]
    zml = A.f32(TT)
    NSET = 2
    km = [A.f32(TT) for _ in range(NSET)]
    rm = [A.f32(TT) for _ in range(NSET)]
    vm = [A.bf(TT) for _ in range(NSET)]
    Tm1 = [A.f32(TT) for _ in range(8)]
    Tm = [Tm1 for _ in range(NSET)]
    sq1 = A.bf(TT)
    sq = [sq1 for _ in range(NSET)]
    AR3 = [A.bf(NCH * 256).rearrange("p (c n) -> p c n", c=NCH) for _ in range(NSET)]
    AR = [a.rearrange("p c (a t) -> p c a t", a=2) for a in AR3]
    bbar = [A.bf(TT) for _ in range(NSET)]
    kbar = [A.bf(TT) for _ in range(NSET)]
    rkb = [A.bf(TT) for _ in range(NSET)]
    tok = [[A.bf(384) for _ in range(NCH)] for _ in range(NSET)]
    gam = [A.f32(NCH) for _ in range(NSET)]
    Ub = [[A.bf(128) for _ in range(NCH)] for _ in range(NSET)]
    NSLOT = NSET * NCH * 2
    M12 = [A.bf(512) for _ in range(NSLOT)]
    M3 = [A.bf(128) for _ in range(NSLOT)]
    ZPP = [[A.bf(384) for _ in range(2)] for _ in range(NSLOT)]
    ZS = [A.bf(128) for _ in range(NSLOT)]
    Zf = [A.bf(128) for _ in range(NSLOT)]
    W1p = [[A.bf(128) for _ in range(NCH)] for _ in range(NSET)]
    catT = A.bf(8 * TT).rearrange("p (k t) -> p k t", k=8)
    Yb = [A.f32(128) for _ in range(2)]
    Ycat = [A.bf(128) for _ in range(2)]
    stat = [A.f32(16) for _ in range(2)]
    pbuf = [A.f32(16 + TT) for _ in range(2)]
    swin2 = [A.f32(16 + TT) for _ in range(2)]
    swin = [swin2[0], swin2[1], swin2[0], swin2[1]]
    invc = [A.f32(TT) for _ in range(2)]
    posf = A.f32(TT)
    diffT = [A.bf(TT) for _ in range(2)]
    mtmp = A.f32(TT)
    xres = [A.f32(D) for _ in range(1)]
    lnst = [A.f32(24) for _ in range(1)]
    print("mixer arena peak", A.peak, A.peak * 4 / 1024, "KB")

    banks = [ps[:, b * 512:(b + 1) * 512] for b in range(8)]
    bctr = [0]
    ctr = {"zs": 0, "py": 0}

    def newbank():
        b = bctr[0]
        bctr[0] = (b + 1) % 8
        return banks[b], ("B", b)

    def nxt(kind, n):
        i = ctr[kind]
        ctr[kind] = (i + 1) % n
        return i

    def TRm(out, in_, r, bk):
        MM(out, in_, ident, True, True, list(r) + ["ident"], [bk])

    for p_ in range(2):
        P.op("pool", lambda e, b=pbuf[p_]: e.memset(b, 0.0), [], [("pbuf", p_)])

    DMA("pool", xs[start_tile % 2], x_ext[start_tile * TT:(start_tile + 1) * TT, :].rearrange("(n p) d -> p n d", p=128),
        [], [("xs", start_tile % 2)])

    def inproj(col):
        bk_ap, bk = newbank()
        out = bk_ap[:, 0:TT]
        for kc in range(8):
            MM(out, w_in[:, kc, col:col + 128], xT[:, kc, :], kc == 0, kc == 7, ["w_in", ("xT", kc // 2)], [bk])
        return out, bk

    def shiftmix(zp, zkey, gi, out, okey):
        zi = nxt("zs", 2)
        z = zs[zi]
        ACT(z[:, 1:TT + 1], zp, AF.Copy, [], [("zsb", zi), zkey])
        CP("pool", z[:, 0:1], carry[:, gi:gi + 1], [("carry", gi)], [("zsc", zi)])
        CP("pool", carry[:, gi:gi + 1], z[:, TT:TT + 1], [("zsb", zi)], [("carry", gi)])
        TTo("dve", dtmp[zi], z[:, 0:TT], zp, ALU.subtract, [("zsb", zi), ("zsc", zi)], [("dtmp", zi), zkey])
        STT(out, dtmp[zi], pp[:, PP_MU + gi:PP_MU + gi + 1], zp, ALU.mult, ALU.add, [("dtmp", zi), "pp"], [okey, zkey])

    import os
    evq = [0]

    def evac_eng():
        evq[0] += 1
        return "act" if evq[0] % 2 else "dve"

    try:
        for tt in range(start_tile, NTILE):
            own = tt >= OWN0
            poolt = tt >= OWN0 - 1
            xsl = tt % 2
            chk("setup", [(ident, "ident"), (MU, "MU"), (SL, "SL"), (bones, "bones"), (pp, "pp")])
            if tt + 1 < NTILE:
                DMA("pool", xs[(tt + 1) % 2],
                    x_ext[(tt + 1) * TT:(tt + 2) * TT, :].rearrange("(n p) d -> p n d", p=128), [], [("xs", (tt + 1) % 2)])
            for j in range(4):
                bk_ap, bk = newbank()
                for kk in range(2):
                    kc = 2 * j + kk
                    for blk in range(NCH):
                        TRm(bk_ap[:, kk * 256 + blk * 128: kk * 256 + (blk + 1) * 128],
                            xs[xsl][:, blk, kc * 128:(kc + 1) * 128], [("xs", xsl)], bk)
                CP(evac_eng(), xT[:, 2 * j:2 * j + 2, :], bk_ap.rearrange("p (a t) -> p a t", a=2), [], [("xT", j), bk])
            chk("xT", [(xT[:, 0, :], ("xT", 0)), (xT[:, 7, :], ("xT", 3))])
            zp, zk = inproj(2560)
            shiftmix(zp, zk, 18, zml, "zml")
            ACT(zml[0:64, :], zml[0:64, :], AF.Sigmoid, ["zml"], ["zml"], scale=2.0)
            TS("dve", lor[0:64, :], zml[0:64, :], 2.0, -1.0, ALU.mult, ALU.add, ["zml"], ["lor"])
            CP("pool", lor[64:128, :], zml[64:128, :], ["zml"], ["lor"])
            if own:
                zp, zk = inproj(2688)
                shiftmix(zp, zk, 19, zml, "zml")
                ACT(sgb, zml, AF.Sigmoid, ["zml"], ["sgb"])
            chk("lor", [(zml, "zml"), (lor, "lor"), (sgb, "sgb")])
            if poolt:
                for q in range(2):
                    zp, zk = inproj(128 * q)
                    pb = pbuf[q]
                    ACT(pb[:, 16:16 + TT], zp, AF.Copy, [], [("pbuf", q), zk])
                    if own:
                        n = 16 + TT
                        s2, s4, s8, s16 = swin
                        TTo("pool", s2[:, 1:n], pb[:, 1:n], pb[:, 0:n - 1], ALU.add, [("pbuf", q)], ["s2"])
                        TTo("pool", s4[:, 3:n], s2[:, 3:n], s2[:, 1:n - 2], ALU.add, ["s2"], ["s4"])
                        if q == 1:
                            TTo("pool", s8[:, 7:n], s4[:, 7:n], s4[:, 3:n - 4], ALU.add, ["s4"], ["s2"])
                            TTo("pool", s16[:, 15:n], s8[:, 15:n], s8[:, 7:n - 8], ALU.add, ["s2"], ["s4"])
                        wins = (2.0, 4.0) if q == 0 else (8.0, 16.0)
                        srcs = (s2, s4) if q == 0 else (s8, s16)
                        skeys = ("s2", "s4")
                        base = (tt - OWN0) * TT + 1
                        P.op("pool", lambda e, b=base: e.iota(posf, pattern=[[1, TT]], base=b, channel_multiplier=0,
                                                                allow_small_or_imprecise_dtypes=True), [], ["posf"])
                        for hf in range(2):
                            rows = slice(64 * hf, 64 * hf + 64)
                            TS("dve", invc[q][rows, :], posf[rows, :], pos0[rows, 0:1], wins[hf], ALU.add, ALU.min,
                               ["posf", "pos0"], [("invc", q)])
                        P.op("dve", lambda e, o=invc[q]: e.reciprocal(out=o, in_=o), [("invc", q)], [("invc", q)])
                        for hf in range(2):
                            rows = slice(64 * hf, 64 * hf + 64)
                            TTo("pool", mtmp[rows, :], srcs[hf][rows, 16:16 + TT], invc[q][rows, :], ALU.mult,
                                [skeys[hf], ("invc", q)], ["mtmp"])
                        TTo("pool", diffT[q], mtmp, pb[:, 16:16 + TT], ALU.subtract, ["mtmp", ("pbuf", q)], [("diffT", q)])
                        bk_ap, bk = newbank()
                        MM(bk_ap[:, 0:TT], poolbd[:, q, :], diffT[q], True, True, ["poolbd", ("diffT", q)], [bk])
                        ACT(catT[:, q, :], bk_ap[:, 0:TT], AF.Copy, ["pp"], [("catT", q), bk],
                            scale=pp[:, PP_PS + q:PP_PS + q + 1])
                    CP("pool", pb[:, 0:16], pb[:, TT:TT + 16], [], [("pbuf", q)])
            def bulk(p):
                st = p % NSET
                T = Tm[st]
                zp, zk = inproj(1024 + 128 * p)
                shiftmix(zp, zk, 6 + p, km[st], ("km", st))
                zp, zk = inproj(1792 + 128 * p)
                shiftmix(zp, zk, 12 + p, vm[st], ("vm", st))
                if own:
                    zp, zk = inproj(256 + 128 * p)
                    shiftmix(zp, zk, p, rm[st], ("rm", st))
                bk_ap, bk = newbank()
                MM(bk_ap[:, 0:TT], loraup[0:64, 128 * p:128 * p + 128], lor[0:64, :], True, True, ["loraup", "lor"], [bk])
                ACT(T[0], bk_ap[:, 0:TT], AF.Sigmoid, ["pp"], [("T0",), bk], bias=pp[:, PP_W0 + p:PP_W0 + p + 1])
                bk_ap, bk = newbank()
                MM(bk_ap[:, 0:TT], loraup[64:128, 128 * p:128 * p + 128], lor[64:128, :], True, True, ["loraup", "lor"], [bk])
                ACT(T[1], bk_ap[:, 0:TT], AF.Sigmoid, ["pp"], [("T1",), bk], bias=pp[:, PP_A0 + p:PP_A0 + p + 1])
                TS("pool", T[2], km[st], pp[:, PP_KK + p:PP_KK + p + 1], None, ALU.mult, None, [("km", st), "pp"], [("T2",)])
                TTo("pool", sq[st], T[2], T[2], ALU.mult, [("T2",)], [("sq",)])
                bk_ap, bk = newbank()
                MM(bk_ap[:, 0:TT], bones, sq[st], True, True, ["bones", ("sq",)], [bk])
                TS("dve", T[3], bk_ap[:, 0:TT], 1e-24, None, ALU.max, None, [], [("T3",), bk])
                ACT(T[3], T[3], AF.Ln, [("T3",)], [("T3",)])
                ACT(T[3], T[3], AF.Exp, [("T3",)], [("T3",)], scale=-0.5)
                TTo("pool", T[2], T[2], T[3], ALU.mult, [("T2",), ("T3",)], [("T2",)])
                TTo("pool", T[4], T[2], T[1], ALU.mult, [("T2",), ("T1",)], [("T4",)])
                TS("dve", T[5], T[1], pp[:, PP_KA + p:PP_KA + p + 1], omk[:, p:p + 1], ALU.mult, ALU.add,
                   [("T1",), "pp", "omk"], [("T5",)])
                TTo("pool", T[5], km[st], T[5], ALU.mult, [("km", st), ("T5",)], [("T5",)])
                for c in range(NCH):
                    cs = slice(c * 128, (c + 1) * 128)
                    P.op("dve", lambda e, o=T[6][:, cs], i=T[0][:, cs]: e.tensor_tensor_scan(
                        out=o, data0=i, data1=zeros, initial=0.0, op0=ALU.add, op1=ALU.add),
                        [("T0",), "zeros"], [("T6",)])
                ACT(T[7], T[6], AF.Exp, [("T6",)], [("T7",)], scale=SC)
                ACT(T[3], T[6], AF.Exp, [("T6",), ("T2",)], [("T3",)], scale=-SC)
                T2v = T[2].rearrange("p (c t) -> p c t", c=NCH)
                T7v = T[7].rearrange("p (c t) -> p c t", c=NCH)
                ark = ("AR", st)
                STT(AR[st][:, :, 0, 1:128], T2v[:, :, 1:128], -1.0, T7v[:, :, 0:127], ALU.mult, ALU.mult,
                    [("T2",), ("T7",)], [ark])
                TS("pool", AR[st][:, :, 0, 0:1], T2v[:, :, 0:1], -1.0, None, ALU.mult, None, [("T2",)], [ark])
                TTo("pool", bbar[st], T[4], T[3], ALU.mult, [("T4",), ("T3",)], [("bbar", st)])
                TTo("dve", kbar[st], T[5], T[3], ALU.mult, [("T5",), ("T3",)], [("kbar", st)])
                CP("pool", gam[st].rearrange("p (c o) -> p c o", o=1), T7v[:, :, 127:128], [("T7",)], [("gam", st)])
                if own:
                    TTo("pool", AR[st][:, :, 1, :], rm[st].rearrange("p (c t) -> p c t", c=NCH), T7v, ALU.mult,
                        [("rm", st), ("T7",)], [ark])
                    TTo("pool", rkb[st], rm[st], T[5], ALU.mult, [("rm", st), ("T5",)], [("rkb", st)])
                for c in range(NCH):
                    cs = slice(c * 128, (c + 1) * 128)
                    bk_ap, bk = newbank()
                    for j, (src, skey) in enumerate(((bbar[st], ("bbar", st)), (kbar[st], ("kbar", st)), (vm[st], ("vm", st)))):
                        TRm(bk_ap[:, j * 128:(j + 1) * 128], src[:, cs], [skey], bk)
                    CP(evac_eng(), tok[st][c], bk_ap[:, 0:384], [], [("tok", st, c), bk])

            def units(p):
                st = p % NSET
                return [(c, e2, (st * NCH + c) * 2 + e2) for c in range(NCH) for e2 in range(2)]

            def abuild(p):
                st = p % NSET
                ark = ("AR", st)
                wA = 256 if own else 128
                for c in range(NCH):
                    cs = slice(c * 128, (c + 1) * 128)
                    bks = [newbank() for _ in range(2)]
                    for (src, skey, off) in ((bbar[st], ("bbar", st), 0), (kbar[st], ("kbar", st), 256)):
                        for e2 in range(2):
                            rows = slice(64 * e2, 64 * e2 + 64)
                            MM(bks[e2][0][:, off:off + wA], src[rows, cs], AR3[st][rows, c, 0:wA], True, True,
                               [skey, ark], [bks[e2][1]])
                    for e2 in range(2):
                        s = (st * NCH + c) * 2 + e2
                        bk_ap, bk = bks[e2]
                        if own:
                            TTo("dve", M12[s], bk_ap, MU, ALU.mult, ["MU"], [("M12", s), bk])
                        else:
                            v3 = lambda a: a.rearrange("p (a b) -> p a b", a=2)[:, :, 0:128]
                            TTo("dve", v3(M12[s]), v3(bk_ap), v3(MU), ALU.mult, ["MU"], [("M12", s), bk])
                for (c, e2, s) in units(p):
                    cs = slice(c * 128, (c + 1) * 128)
                    rows = slice(64 * e2, 64 * e2 + 64)
                    bk_ap, bk = newbank()
                    MM(bk_ap[:, 0:128], AR3[st][rows, c, 0:128], bbar[st][rows, cs], True, True, [ark, ("bbar", st)], [bk])
                    TTo("dve", M3[s], bk_ap[:, 0:128], SL, ALU.mult, ["SL"], [("M3", s), bk])

            curz = {}

            def chain_iter(p, it):
                for (c, e2, s) in units(p):
                    zk_ = lambda i: ("ZPP", s, i)
                    if it == 0:
                        Nn = M12[s][:, 0:128]
                        bk_ap, bk = newbank()
                        MM(bk_ap[:, 0:128], M3[s], Nn, True, True, [("M3", s), ("M12", s)], [bk])
                        MM(bk_ap[:, 128:256], Nn, M3[s], True, True, [("M3", s), ("M12", s)], [bk])
                        CP("pool", ZPP[s][0][:, 0:128], Nn, [("M12", s)], [zk_(0)])
                        ACT(ZPP[s][0][:, 128:384], bk_ap[:, 0:256], AF.Copy, [], [zk_(0), bk])
                        curz[s] = 0
                        continue
                    cur = curz[s]
                    nx = 1 - cur
                    Z = ZPP[s][cur]
                    TTo("dve", ZS[s], Z[:, 0:128], Z[:, 128:256], ALU.add, [zk_(cur)], [("ZS", s)])
                    bk_ap, bk = newbank()
                    if it < 6:
                        MM(bk_ap[:, 0:256], Z[:, 256:384], Z[:, 0:256], True, True, [zk_(cur)], [bk])
                        MM(bk_ap[:, 256:384], Z[:, 128:256], Z[:, 256:384], True, True, [zk_(cur)], [bk])
                        TTo("dve", ZPP[s][nx][:, 0:128], bk_ap[:, 0:128], ZS[s], ALU.add, [("ZS", s)], [zk_(nx), bk])
                        ACT(ZPP[s][nx][:, 128:384], bk_ap[:, 128:384], AF.Copy, [], [zk_(nx), bk])
                    else:
                        MM(bk_ap[:, 0:128], Z[:, 256:384], Z[:, 0:128], True, True, [zk_(cur)], [bk])
                        TTo("dve", Zf[s], bk_ap[:, 0:128], ZS[s], ALU.add, [("ZS", s)], [("Zf", s), bk])
                    curz[s] = nx

            def seq_step(p, k):
                if k >= 3 * NCH:
                    return
                st = p % NSET
                ark = ("AR", st)
                c = k // 3
                step = k % 3
                cs = slice(c * 128, (c + 1) * 128)
                tkk = ("tok", st, c)
                hk = ("Hb", p)
                uk = ("U", st, c)
                wk = ("W1p", st, c)
                sl_ = [(st * NCH + c) * 2 + e2 for e2 in range(2)]
                if step == 0:
                    bk_ap, bk = newbank()
                    for e2 in range(2):
                        rows = slice(64 * e2, 64 * e2 + 64)
                        s = sl_[e2]
                        vt = tok[st][c][:, 256 + 64 * e2:256 + 64 * e2 + 64]
                        oo = bk_ap[:, 64 * e2:64 * e2 + 64]
                        MM(oo, AR3[st][rows, c, 0:128], Hb[rows, p, :], True, False, [ark, hk], [bk])
                        MM(oo, M12[s][:, 256:384], vt, False, True, [("M12", s), tkk], [bk])
                    ACT(W1p[st][c], bk_ap[:, 0:128], AF.Copy, [], [wk, bk])
                    return
                if step == 1:
                    bk_ap, bk = newbank()
                    for e2 in range(2):
                        s = sl_[e2]
                        MM(bk_ap[:, 64 * e2:64 * e2 + 64], Zf[s], W1p[st][c][:, 64 * e2:64 * e2 + 64], True, True,
                           [("Zf", s), wk], [bk])
                    TTo("dve", Ub[st][c], bk_ap[:, 0:128], W1p[st][c], ALU.add, [wk], [uk, bk])
                    return
                bk_ap, bk = newbank()
                if own:
                    for e2 in range(2):
                        rows = slice(64 * e2, 64 * e2 + 64)
                        s = sl_[e2]
                        vt = tok[st][c][:, 256 + 64 * e2:256 + 64 * e2 + 64]
                        oo = bk_ap[:, 64 * e2:64 * e2 + 64]
                        MM(oo, AR3[st][rows, c, 128:256], Hb[rows, p, :], True, False, [ark, hk], [bk])
                        MM(oo, M12[s][:, 128:256], Ub[st][c][:, 64 * e2:64 * e2 + 64], False, False, [("M12", s), uk], [bk])
                        MM(oo, M12[s][:, 384:512], vt, False, True, [("M12", s), tkk], [bk])
                hp = bk_ap[:, 128:256]
                MM(hp, tok[st][c][:, 0:128], Ub[st][c], True, False, [tkk, uk], [bk])
                MM(hp, tok[st][c][:, 128:256], tok[st][c][:, 256:384], False, True, [tkk], [bk])
                if own:
                    MM(bk_ap[:, 256:258], rkb[st][:, cs], RK[:, 2 * p:2 * p + 2], True, True, [("rkb", st), "RK"], [bk])
                    MM(bk_ap[:, 384:512], sgb[:, cs], gup[:, 128 * p:128 * p + 128], True, True, ["sgb", "gup"], [bk])
                TTo("dve", tmpH[0:64, :], hp[0:64, 0:64], Hf[0:64, p, :], ALU.add, [("Hf", p)], ["tmpH", bk])
                TTo("dve", tmpH[64:128, :], hp[64:128, 64:128], Hf[64:128, p, :], ALU.add, [("Hf", p)], ["tmpH", bk])
                ACT(Hf[:, p, :], tmpH, AF.Copy, ["tmpH", ("gam", st)], [("Hf", p)], scale=gam[st][:, c:c + 1])
                CP("pool", Hb[:, p, :], Hf[:, p, :], [("Hf", p)], [hk])
                if own:
                    yi = nxt("py", 2)
                    Y = Yb[yi]
                    yk = ("Y", yi)
                    sti = stat[yi]
                    sk_ = ("stat", yi)
                    for e2 in range(2):
                        P.op("dve", lambda e, o=sti[:, 6 * e2:6 * e2 + 6], i=bk_ap[:, 64 * e2:64 * e2 + 64]:
                             e.bn_stats(out=o, in_=i), [], [sk_, bk])
                        P.op("dve", lambda e, o=sti[:, 12 + 2 * e2:14 + 2 * e2], i=sti[:, 6 * e2:6 * e2 + 6]:
                             e.bn_aggr(out=o, in_=i), [sk_], [sk_])
                    for e2 in range(2):
                        ACT(sti[:, 13 + 2 * e2:14 + 2 * e2], sti[:, 13 + 2 * e2:14 + 2 * e2], AF.Ln,
                            [sk_, "epsx"], [sk_], bias=epsx[:, 0:1])
                        ACT(sti[:, 13 + 2 * e2:14 + 2 * e2], sti[:, 13 + 2 * e2:14 + 2 * e2], AF.Exp,
                            [sk_], [sk_], scale=-0.5)
                    for e2 in range(2):
                        TS("dve", Y[:, 64 * e2:64 * e2 + 64], bk_ap[:, 64 * e2:64 * e2 + 64],
                           sti[:, 12 + 2 * e2:13 + 2 * e2], sti[:, 13 + 2 * e2:14 + 2 * e2], ALU.subtract, ALU.mult,
                           [sk_], [yk, bk])
                    TTo("pool", Y, Y, bc[:, BC_LNXG + 128 * p:BC_LNXG + 128 * p + 128], ALU.mult, [yk, "bc"], [yk])
                    TTo("pool", Y, Y, bc[:, BC_LNXB + 128 * p:BC_LNXB + 128 * p + 128], ALU.add, [yk, "bc"], [yk])
                    for e2 in range(2):
                        STT(Y[:, 64 * e2:64 * e2 + 64], tok[st][c][:, 256 + 64 * e2:256 + 64 * e2 + 64],
                            bk_ap[:, 256 + e2:257 + e2], Y[:, 64 * e2:64 * e2 + 64], ALU.mult, ALU.add,
                            [tkk, yk], [yk, bk])
                    TTo("dve", Ycat[yi], Y, bk_ap[:, 384:512], ALU.mult, [yk], [("Ycat", yi), bk])
                    bk2_ap, bk2 = newbank()
                    TRm(bk2_ap[:, 0:128], Ycat[yi], [("Ycat", yi)], bk2)
                    ACT(catT[:, 2 + p, cs], bk2_ap[:, 0:128], AF.Copy, [], [("catT", 2 + p), bk2])

            for p in range(7):
                if p < 6:
                    bulk(p)
                    abuild(p)
                for it in range(7):
                    if p < 6:
                        chain_iter(p, it)
                    if p >= 1:
                        seq_step(p - 1, it)
            chk("seq", [(Hf.rearrange("p a b -> p (a b)"), ("Hf", 5)), (catT[:, 0, :], ("catT", 0)), (catT[:, 1, :], ("catT", 1)),
                        (catT[:, 2, :], ("catT", 2)), (catT[:, 7, :], ("catT", 7))])
            if own:
                for blk in range(NCH):
                    cs = slice(blk * 128, (blk + 1) * 128)
                    xr = xres[0]
                    xk = ("xres", 0)
                    row0 = tt * TT + blk * 128
                    DMA("sp", xr, x_ext[row0:row0 + 128, :], [], [xk])
                    for dh in range(2):
                        bk_ap, bk = newbank()
                        for kc in range(8):
                            MM(bk_ap, catT[:, kc, cs], w_out[:, kc, dh * 512:(dh + 1) * 512], kc == 0, kc == 7,
                               [("catT", kc), "w_out"], [bk])
                        STT(xr[:, dh * 512:(dh + 1) * 512], xr[:, dh * 512:(dh + 1) * 512], ALPHA, bk_ap, ALU.mult, ALU.add,
                            [], [xk, bk])
                    ls = lnst[0]
                    lk = ("lnst", 0)
                    for dh in range(2):
                        P.op("dve", lambda e, o=ls[:, 6 * dh:6 * dh + 6], i=xr[:, dh * 512:(dh + 1) * 512]:
                             e.bn_stats(out=o, in_=i), [xk], [lk])
                    P.op("dve", lambda e, o=ls[:, 12:14], i=ls[:, 0:12]: e.bn_aggr(out=o, in_=i), [lk], [lk])
                    ACT(ls[:, 13:14], ls[:, 13:14], AF.Ln, [lk, "eps1"], [lk], bias=eps1[:, 0:1])
                    ACT(ls[:, 13:14], ls[:, 13:14], AF.Exp, [lk], [lk], scale=-0.5)
                    TS("dve", xr, xr, ls[:, 12:13], ls[:, 13:14], ALU.subtract, ALU.mult, [xk, lk], [xk])
                    TTo("pool", xr, xr, bc[:, BC_LN1G:BC_LN1G + D], ALU.mult, [xk, "bc"], [xk])
                    TTo("pool", xr, xr, bc[:, BC_LN1B:BC_LN1B + D], ALU.add, [xk, "bc"], [xk])
                    r0 = (tt - OWN0) * TT + blk * 128
                    hdst = out_d if (debug_h or not do_moe) else hbuf
                    DMA("sp", hdst[r0:r0 + 128, :], xr, [xk], [("hbuf", r0 // 128)])
    except StopBuild:
        pass

    if do_moe and not debug_h:
        build_moe(nc, P, A, ps, hbuf, out_d, bc2_d, router_d, eg_d, eu_d, ed_d,
                  (TTo, TS, STT, ACT, CP, MM, DMA, newbank, ident))

    fin = [(s, v) for s, v in P.dma_val.items()]
    waits = P._waits("sp", fin)
    if waits:
        P.q["sp"].append((waits, None, None))

    nc._prog_log = P.log
    names = P.sem_names()
    semh = {}
    cms = []
    for n in names:
        cm = nc.semaphore(n)
        cms.append(cm)
        semh[n] = cm.__enter__()
    P.emit(semh)
    for cm in reversed(cms):
        cm.__exit__(None, None, None)
    psum_t.__exit__(None, None, None)
    arena_t.__exit__(None, None, None)
    return nc


def build_moe(nc, P, A, ps, hbuf, out_d, bc2_d, router_d, eg_d, eu_d, ed_d, H):
    (TTo, TS, STT, ACT, CP, MM, DMA, newbank, ident) = H
    P.barrier()
    A.off = 0
    NT_ = OWN // 128
    identm = A.bf(128)
    onesm = A.f32(128)
    bc2 = A.f32(BC2_N)
    eps2 = A.f32(2)
    routf = A.f32(8 * 36)
    rout = A.bf(8 * 36).rearrange("p (k n) -> p k n", k=8)
    yacc = A.f32(NT_ * D).rearrange("p (i d) -> p i d", i=NT_)
    hT = A.bf(8 * OWN).rearrange("p (k t) -> p k t", k=8)
    Wt = A.f32(NT_ * 32).rearrange("p (i e) -> p i e", i=NT_)
    hld = [A.f32(D) for _ in range(2)]
    hbf = [A.bf(D) for _ in range(2)]
    Wg = [A.bf(8 * 256).rearrange("p (k n) -> p k n", k=8) for _ in range(2)]
    Wu = [A.bf(8 * 256).rearrange("p (k n) -> p k n", k=8) for _ in range(2)]
    Wd = [A.bf(2 * D).rearrange("p (k n) -> p k n", k=2) for _ in range(2)]
    hid = [A.bf(2 * OWN).rearrange("p (k t) -> p k t", k=2) for _ in range(2)]
    sgt = [A.bf(512) for _ in range(2)]
    rt = A.f32(256)
    lnst = A.f32(24)
    print("moe arena peak", A.off, A.off * 4 / 1024, "KB")

    P.op("pool", lambda e: e.memset(onesm, 1.0), [], ["onesm"])
    P.op("pool", lambda e: e.affine_select(out=identm, in_=onesm, pattern=[[-1, 128]], compare_op=ALU.is_equal,
                                           fill=0.0, base=0, channel_multiplier=1), ["onesm"], ["identm"])
    P.op("pool", lambda e: e.memset(eps2, LN_EPS), [], ["eps2"])
    DMA("sp", bc2, bc2_d, [], ["bc2"])
    DMA("sp", routf.rearrange("p (k n) -> p k n", k=8), router_d.rearrange("(k p) n -> p k n", p=128), [], ["routf"])
    CP("pool", rout.rearrange("p k n -> p (k n)"), routf, ["routf"], ["rout"])

    def load_w(e):
        b = e % 2
        DMA("pool", Wg[b], eg_d[e].rearrange("(k p) n -> p k n", p=128), [], [("Wg", b)])
        DMA("pool", Wu[b], eu_d[e].rearrange("(k p) n -> p k n", p=128), [], [("Wu", b)])
        DMA("pool", Wd[b], ed_d[e].rearrange("(k p) n -> p k n", p=128), [], [("Wd", b)])

    load_w(0)
    for i in range(NT_):
        hb = i % 2
        hk = ("hld", hb)
        DMA("sp", hld[hb], hbuf[i * 128:(i + 1) * 128, :], [("hbuf", i)], [hk])
        ACT(yacc[:, i, :], hld[hb], AF.Copy, [hk], [("yacc", i)], scale=ALPHA)
        CP("pool", hbf[hb], hld[hb], [hk], [("hbf", hb)])
        for j in range(2):
            bk_ap, bk = newbank()
            for kk in range(4):
                kc = 4 * j + kk
                MM(bk_ap[:, kk * 128:(kk + 1) * 128], hbf[hb][:, kc * 128:(kc + 1) * 128], identm, True, True,
                   [("hbf", hb), "identm"], [bk])
            CP("act" if j == 0 else "dve", hT[:, 4 * j:4 * j + 4, i * 128:(i + 1) * 128],
               bk_ap.rearrange("p (a t) -> p a t", a=4), [], [("hT", i), bk])
        bk_ap, bk = newbank()
        for kc in range(8):
            MM(bk_ap[:, 0:36], hT[:, kc, i * 128:(i + 1) * 128], rout[:, kc, :], kc == 0, kc == 7, [("hT", i), "rout"], [bk])
        lg = rt[:, 0:36]
        rk_ = "rt"
        TTo("dve", lg, bk_ap[:, 0:36], bc2[:, BC2_RB:BC2_RB + 36], ALU.add, ["bc2"], [rk_, bk])
        gl = rt[:, 0:4]
        gmax = rt[:, 40:41]
        ngmax = rt[:, 41:42]
        gsum = rt[:, 42:43]
        gexp = rt[:, 44:48]
        goh = rt[:, 48:52]
        esel = rt[:, 56:64]
        m8 = rt[:, 64:72]
        w1 = rt[:, 72:73]
        w2 = rt[:, 73:74]
        tmp8 = rt[:, 80:88]
        we8 = rt[:, 88:96]
        gsc = rt[:, 96:100]
        P.op("dve", lambda e: e.tensor_reduce(out=gmax, in_=gl, axis=mybir.AxisListType.X, op=ALU.max), [rk_], [rk_])
        TS("dve", ngmax, gmax, -1.0, None, ALU.mult, None, [rk_], [rk_])
        ACT(gexp, gl, AF.Exp, [rk_], [rk_], bias=ngmax)
        P.op("dve", lambda e: e.tensor_reduce(out=gsum, in_=gexp, axis=mybir.AxisListType.X, op=ALU.add), [rk_], [rk_])
        P.op("dve", lambda e: e.reciprocal(out=gsum, in_=gsum), [rk_], [rk_])
        TS("dve", goh, gl, gmax, None, ALU.is_equal, None, [rk_], [rk_])
        TS("dve", esel, rt[:, 4:12], goh[:, 0:1], None, ALU.mult, None, [rk_], [rk_])
        for g in range(1, 4):
            STT(esel, rt[:, 4 + 8 * g:12 + 8 * g], goh[:, g:g + 1], esel, ALU.mult, ALU.add, [rk_], [rk_])
        P.op("dve", lambda e: e.max(out=m8, in_=esel), [rk_], [rk_])
        TTo("dve", w2, m8[:, 1:2], m8[:, 0:1], ALU.subtract, [rk_], [rk_])
        ACT(w2, w2, AF.Exp, [rk_], [rk_])
        TS("dve", w1, w2, 1.0, None, ALU.add, None, [rk_], [rk_])
        P.op("dve", lambda e: e.reciprocal(out=w1, in_=w1), [rk_], [rk_])
        TTo("dve", w2, w2, w1, ALU.mult, [rk_], [rk_])
        TS("dve", we8, esel, m8[:, 0:1], w1, ALU.is_equal, ALU.mult, [rk_], [rk_])
        TS("dve", tmp8, esel, m8[:, 1:2], w2, ALU.is_equal, ALU.mult, [rk_], [rk_])
        TTo("dve", we8, we8, tmp8, ALU.add, [rk_], [rk_])
        TS("dve", gsc, goh, gsum, None, ALU.mult, None, [rk_], [rk_])
        for g in range(4):
            TS("dve", Wt[:, i, 8 * g:8 * g + 8], we8, gsc[:, g:g + 1], None, ALU.mult, None, [rk_], [("Wt", i)])

    for e in range(32):
        b = e % 2
        if e + 1 < 32:
            load_w(e + 1)
        hd = hid[b]
        for tb in range(4):
            ts_ = slice(tb * 512, (tb + 1) * 512)
            for half in range(2):
                g_ap, gk = newbank()
                for kc in range(8):
                    MM(g_ap, Wg[b][:, kc, half * 128:(half + 1) * 128], hT[:, kc, ts_], kc == 0, kc == 7,
                       [("Wg", b)] + [("hT", 4 * tb + q) for q in range(4)], [gk])
                u_ap, uk_ = newbank()
                for kc in range(8):
                    MM(u_ap, Wu[b][:, kc, half * 128:(half + 1) * 128], hT[:, kc, ts_], kc == 0, kc == 7,
                       [("Wu", b)] + [("hT", 4 * tb + q) for q in range(4)], [uk_])
                si = (tb * 2 + half) % 2
                ACT(sgt[si], g_ap, AF.Silu, [], [("sgt", si), gk])
                TTo("dve", hd[:, half, ts_], sgt[si], u_ap, ALU.mult, [("sgt", si)], [("hid", b, tb), uk_])
        for i in range(NT_):
            for dh in range(2):
                y_ap, yk_ = newbank()
                for half in range(2):
                    MM(y_ap, hd[:, half, i * 128:(i + 1) * 128], Wd[b][:, half, dh * 512:(dh + 1) * 512], half == 0, half == 1,
                       [("hid", b, i // 4), ("Wd", b)], [yk_])
                STT(yacc[:, i, dh * 512:(dh + 1) * 512], y_ap, Wt[:, i, e:e + 1], yacc[:, i, dh * 512:(dh + 1) * 512],
                    ALU.mult, ALU.add, [("Wt", i)], [("yacc", i), yk_])
    for i in range(NT_):
        y = yacc[:, i, :]
        yk = ("yacc", i)
        lk = "lnst2"
        for dh in range(2):
            P.op("dve", lambda e, o=lnst[:, 6 * dh:6 * dh + 6], i_=y[:, dh * 512:(dh + 1) * 512]:
                 e.bn_stats(out=o, in_=i_), [yk], [lk])
        P.op("dve", lambda e: e.bn_aggr(out=lnst[:, 12:14], in_=lnst[:, 0:12]), [lk], [lk])
        ACT(lnst[:, 13:14], lnst[:, 13:14], AF.Ln, [lk, "eps2"], [lk], bias=eps2[:, 0:1])
        ACT(lnst[:, 13:14], lnst[:, 13:14], AF.Exp, [lk], [lk], scale=-0.5)
        TS("dve", y, y, lnst[:, 12:13], lnst[:, 13:14], ALU.subtract, ALU.mult, [yk, lk], [yk])
        TTo("pool", y, y, bc2[:, BC2_LN2G:BC2_LN2G + D], ALU.mult, [yk, "bc2"], [yk])
        TTo("pool", y, y, bc2[:, BC2_LN2B:BC2_LN2B + D], ALU.add, [yk, "bc2"], [yk])
        DMA("sp", out_d[i * 128:(i + 1) * 128, :], y, [yk], [("out", i)])


def prep_inputs(inputs):
    f = lambda k: np.ascontiguousarray(np.asarray(inputs[k], dtype=np.float32)[0])
    x = np.asarray(inputs["x"], dtype=np.float32)
    mu = f("mu_shift")
    pp = np.zeros((128, PP_N), np.float32)
    pp[:, PP_MU:PP_MU + 20] = mu.reshape(20, 128).T
    pp[:, PP_W0:PP_W0 + 6] = f("w0").reshape(6, 128).T
    pp[:, PP_A0:PP_A0 + 6] = f("a0").reshape(6, 128).T
    pp[:, PP_KK:PP_KK + 6] = f("k_k").reshape(6, 128).T
    pp[:, PP_KA:PP_KA + 6] = f("k_a").reshape(6, 128).T
    pp[:, PP_PS:PP_PS + 2] = f("pool_scale").reshape(2, 128).T
    bc = np.zeros((128, BC_N), np.float32)
    bc[:, BC_LNXG:BC_LNXG + 768] = f("lnx_g")[None, :]
    bc[:, BC_LNXB:BC_LNXB + 768] = f("lnx_b")[None, :]
    bc[:, BC_LN1G:BC_LN1G + D] = f("ln1_g")[None, :]
    bc[:, BC_LN1B:BC_LN1B + D] = f("ln1_b")[None, :]
    bc2 = np.zeros((128, BC2_N), np.float32)
    bc2[:, BC2_LN2G:BC2_LN2G + D] = f("ln2_g")[None, :]
    bc2[:, BC2_LN2B:BC2_LN2B + D] = f("ln2_b")[None, :]
    bc2[:, BC2_RB:BC2_RB + 4] = f("router_group_b")[None, :]
    bc2[:, BC2_RB + 4:BC2_RB + 36] = f("router_expert_b")[None, :]
    rk = f("r_k")
    rkm = np.zeros((128, 12), np.float32)
    for h in range(12):
        e = h % 2
        rkm[64 * e:64 * e + 64, h] = rk[h]
    pw = f("pool_w")
    poolbd = np.zeros((256, 128), np.float32)
    for g in range(4):
        q, e = g // 2, g % 2
        poolbd[128 * q + 64 * e:128 * q + 64 * e + 64, 64 * e:64 * e + 64] = pw[g]
    loraup = np.concatenate([f("w_up"), f("a_up")], axis=0)
    router = np.concatenate([f("router_group"), f("router_expert")], axis=1)
    shared = {
        "w_in": f("w_in"), "pp": pp, "bc": bc, "bc2": bc2, "rkm": rkm, "poolbd": poolbd,
        "loraup": np.ascontiguousarray(loraup), "gup": f("g_up"), "w_out": f("w_out"),
        "router": np.ascontiguousarray(router), "exp_gate": f("exp_gate"), "exp_up": f("exp_up"),
        "exp_down": f("exp_down"),
    }
    in_maps = []
    for c in range(NCORE):
        b, j = c // 4, c % 4
        xe = np.zeros((SEQ, D), np.float32)
        n = OWN * (j + 1)
        xe[SEQ - n:, :] = x[b, 0:n, :]
        m = dict(shared)
        m["x_ext"] = xe
        m["pos0"] = np.full((128, 1), float(OWN * j), np.float32)
        in_maps.append(m)
    return in_maps


def kernel(**inputs):
    in_maps = prep_inputs(inputs)
    nc = build()
    res = run_bass_kernel_spmd(nc, in_maps, core_ids=list(range(NCORE)))
    out = np.zeros((2, SEQ, D), np.float32)
    for c in range(NCORE):
        b, j = c // 4, c % 4
        out[b, OWN * j:OWN * (j + 1), :] = res.results[c]["out"]
    return out
```

```python
import numpy as np
import concourse.bass as bass
import concourse.mybir as mybir
from concourse.bass_utils import run_bass_kernel_spmd

F32 = mybir.dt.float32
BF16 = mybir.dt.bfloat16
ALU = mybir.AluOpType
AF = mybir.ActivationFunctionType

D = 1024
SEQ = 8192
NCORE = 8
OWN = 2048
TT = 256
NTILE = SEQ // TT
OWN0 = (SEQ - OWN) // TT
NCH = TT // 128
DIN = 2816
ALPHA = 2.0 ** 0.25
SC = -float(np.exp(-0.5))
LN_EPS = 1e-5
LNX_EPS = 64e-5
NDMASEM = 6

PP_MU, PP_W0, PP_A0, PP_KK, PP_KA, PP_PS, PP_N = 0, 20, 26, 32, 38, 44, 48
BC_LNXG, BC_LNXB, BC_LN1G, BC_LN1B, BC_N = 0, 768, 1536, 2560, 3584
BC2_LN2G, BC2_LN2B, BC2_RB, BC2_N = 0, 1024, 2048, 2084


class Prog:
    def __init__(self, nc):
        self.nc = nc
        self.engs = {"pe": nc.tensor, "act": nc.scalar, "dve": nc.vector, "pool": nc.gpsimd, "sp": nc.sync}
        self.q = {e: [] for e in self.engs}
        self.cnt = {e: 0 for e in self.engs}
        self.seen = {e: {} for e in self.engs}
        self.lastw = {}
        self.readers = {}
        self.dma_val = {}
        self.dma_rr = {"sp": 0, "pool": 0}
        self.log = []

    def _deps(self, reads, writes, eng=None):
        deps = []
        for k in reads:
            if k in self.lastw:
                deps.append(self.lastw[k])
        for k in writes:
            isbank = isinstance(k, tuple) and k[0] == "B"
            if k in self.lastw:
                d = self.lastw[k]
                if not (isbank and d[0] == eng):
                    deps.append(d)
            for d in self.readers.get(k, ()):
                if not (isbank and d[0] == eng):
                    deps.append(d)
        return deps

    def _waits(self, eng, deps):
        need = {}
        for (s, v) in deps:
            if s == eng and eng == "pe":
                continue
            if need.get(s, 0) < v:
                need[s] = v
        out = []
        for s, v in need.items():
            if self.seen[eng].get(s, 0) >= v:
                continue
            self.seen[eng][s] = v
            out.append((s, v))
        return out

    def _record(self, me, reads, writes):
        for k in reads:
            self.readers.setdefault(k, []).append(me)
        for k in writes:
            self.lastw[k] = me
            self.readers[k] = []

    def op(self, eng, fn, reads=(), writes=()):
        waits = self._waits(eng, self._deps(reads, writes, eng))
        self.cnt[eng] += 1
        me = (eng, self.cnt[eng])
        self.q[eng].append((waits, fn, (eng, 1)))
        self._record(me, reads, writes)
        self.log.append((eng, me, waits, list(reads), list(writes)))
        return me

    def dma(self, eng, fn, reads=(), writes=()):
        i = self.dma_rr[eng]
        self.dma_rr[eng] = (i + 1) % NDMASEM
        s = "dma_%s_%d" % (eng, i)
        prev = self.dma_val.get(s, 0)
        deps = self._deps(reads, writes)
        if prev:
            deps.append((s, prev))
        waits = self._waits(eng, deps)
        self.dma_val[s] = prev + 16
        me = (s, prev + 16)
        self.q[eng].append((waits, fn, (s, 16)))
        self._record(me, reads, writes)
        self.log.append((eng + "-dma", me, waits, list(reads), list(writes)))
        return me

    def barrier(self):
        allsig = [(e, c) for e, c in self.cnt.items() if c > 0 and e != "sp"]
        allsig += [(s, v) for s, v in self.dma_val.items()]
        for eng in self.engs:
            waits = self._waits(eng, [d for d in allsig if d[0] != eng])
            if waits:
                self.q[eng].append((waits, None, None))
        self.lastw = {}
        self.readers = {}

    def sem_names(self):
        names = ["pe", "act", "dve", "pool"]
        for eng in ("sp", "pool"):
            for i in range(NDMASEM):
                names.append("dma_%s_%d" % (eng, i))
        return names

    def emit(self, sems):
        nc = self.nc
        with nc.Block() as block:
            def mk(engname):
                def body(e):
                    for waits, fn, inc in self.q[engname]:
                        for s, v in waits:
                            e.wait_ge(sems[s], v)
                        if fn is not None:
                            ins = fn(e)
                            ins.then_inc(sems[inc[0]], inc[1])
                return body
            block.tensor(mk("pe"))
            block.scalar(mk("act"))
            block.vector(mk("dve"))
            block.gpsimd(mk("pool"))
            block.sync(mk("sp"))


class Arena:
    def __init__(self, ap, cap):
        self.ap = ap
        self.cap = cap
        self.off = 0
        self.peak = 0

    def f32(self, n):
        n2 = (n + 1) // 2 * 2
        o = self.off
        self.off += n2
        self.peak = max(self.peak, self.off)
        assert self.off <= self.cap, ("arena overflow", self.off, self.cap)
        return self.ap[:, o:o + n]

    def bf(self, n):
        u = (n + 1) // 2
        u = (u + 1) // 2 * 2
        o = self.off
        self.off += u
        self.peak = max(self.peak, self.off)
        assert self.off <= self.cap, ("arena overflow", self.off, self.cap)
        return self.ap[:, o:o + u].bitcast(BF16)[:, 0:n]


class StopBuild(Exception):
    pass


def build(start_tile=0, do_moe=True, debug_h=False, stop_at=None):
    nc = bass.Bass("TRN2", target_bir_lowering=False)
    P = Prog(nc)

    def din(name, shape):
        return nc.dram_tensor(name, list(shape), F32, kind="ExternalInput").ap()

    x_ext = din("x_ext", [SEQ, D])
    pos0_d = din("pos0", [128, 1])
    w_in_d = din("w_in", [D, DIN])
    pp_d = din("pp", [128, PP_N])
    bc_d = din("bc", [128, BC_N])
    bc2_d = din("bc2", [128, BC2_N])
    rk_d = din("rkm", [128, 12])
    poolbd_d = din("poolbd", [256, 128])
    loraup_d = din("loraup", [128, 768])
    gup_d = din("gup", [128, 768])
    w_out_d = din("w_out", [D, D])
    router_d = din("router", [D, 36])
    eg_d = din("exp_gate", [32, D, 256])
    eu_d = din("exp_up", [32, D, 256])
    ed_d = din("exp_down", [32, 256, D])
    out_d = nc.dram_tensor("out", [OWN, D], F32, kind="ExternalOutput").ap()
    hbuf = nc.dram_tensor("hbuf", [OWN, D], F32).ap()

    CAP = 45000
    arena_t = nc.sbuf_tensor("arena", [128, CAP], F32)
    psum_t = nc.psum_tensor("ps", [128, 4096], F32)
    arena_h = arena_t.__enter__()
    psum_h = psum_t.__enter__()
    A = Arena(arena_h[:, :], CAP)
    ps = psum_h

    def bank(b):
        return ps[:, b * 512:(b + 1) * 512]

    def TTo(eng, out, in0, in1, op, r, w):
        P.op(eng, lambda e: e.tensor_tensor(out=out, in0=in0, in1=in1, op=op), r, w)

    def TS(eng, out, in0, s1, s2, op0, op1, r, w):
        if op1 is None:
            P.op(eng, lambda e: e.tensor_scalar(out=out, in0=in0, scalar1=s1, scalar2=None, op0=op0), r, w)
        else:
            P.op(eng, lambda e: e.tensor_scalar(out=out, in0=in0, scalar1=s1, scalar2=s2, op0=op0, op1=op1), r, w)

    def STT(out, in0, scalar, in1, op0, op1, r, w):
        P.op("dve", lambda e: e.scalar_tensor_tensor(out=out, in0=in0, scalar=scalar, in1=in1, op0=op0, op1=op1), r, w)

    def ACT(out, in_, func, r, w, bias=None, scale=None):
        kw = {}
        if bias is not None:
            kw["bias"] = bias
        if scale is not None:
            kw["scale"] = scale
        P.op("act", lambda e: e.activation(out=out, in_=in_, func=func, **kw), r, w)

    def CP(eng, out, in_, r, w):
        if eng == "act":
            ACT(out, in_, AF.Copy, r, w)
        else:
            P.op(eng, lambda e: e.tensor_copy(out=out, in_=in_), r, w)

    def MM(out, lhsT, rhs, start, stop, r, w):
        P.op("pe", lambda e: e.matmul(out, lhsT, rhs, start=start, stop=stop), r, w)

    def DMA(eng, out, in_, r, w):
        P.dma(eng, lambda e: e.dma_start(out=out, in_=in_), r, w)


    def chk(name, items):
        if stop_at != name:
            return
        for i, (ap, key) in enumerate(items):
            n = ap.shape[-1]
            scr = dbgscr[:, 0:n]
            CP("pool", scr, ap, [key], ["dbgscr"])
            DMA("sp", out_d[i * 128:(i + 1) * 128, 0:n], scr, ["dbgscr"], [("dbgout", i)])
        raise StopBuild()

    dbgscr = A.f32(1024) if stop_at else None
    ident = A.bf(128)
    MU = A.bf(512)
    SL = A.bf(128)
    bones = A.bf(128)
    onesf = A.f32(128)
    zeros = A.f32(128)
    pp = A.f32(PP_N)
    omk = A.f32(6)
    bc = A.f32(BC_N)
    rkf = A.f32(12)
    RK = A.bf(12)
    poolbd = A.bf(256).rearrange("p (q d) -> p q d", q=2)
    loraup = A.bf(768)
    gup = A.bf(768)
    w_in = A.bf(8 * DIN).rearrange("p (k n) -> p k n", k=8)
    w_out = A.bf(8 * D).rearrange("p (k n) -> p k n", k=8)
    pos0 = A.f32(2)
    carry = A.f32(24)
    Hf = A.f32(6 * 64).rearrange("p (a b) -> p a b", a=6)
    Hb = A.bf(6 * 64).rearrange("p (a b) -> p a b", a=6)
    tmpH = A.f32(64)
    epsx = A.f32(2)
    eps1 = A.f32(2)

    P.op("pool", lambda e: e.memset(onesf, 1.0), [], ["onesf"])
    P.op("pool", lambda e: e.memset(zeros, 0.0), [], ["zeros"])
    P.op("pool", lambda e: e.memset(carry, 0.0), [], ["carry"])
    P.op("pool", lambda e: e.memset(Hf.rearrange("p a b -> p (a b)"), 0.0), [], ["Hf"])
    P.op("pool", lambda e: e.memset(Hb.rearrange("p a b -> p (a b)"), 0.0), [], ["Hb"])
    P.op("pool", lambda e: e.memset(epsx, LNX_EPS), [], ["epsx"])
    P.op("pool", lambda e: e.memset(eps1, LN_EPS), [], ["eps1"])
    P.op("pool", lambda e: e.memset(bones, 0.0), [], ["bones"])
    P.op("pool", lambda e: e.memset(bones[0:64, 0:64], 1.0), [], ["bones"])
    P.op("pool", lambda e: e.memset(bones[64:128, 64:128], 1.0), [], ["bones"])
    P.op("pool", lambda e: e.affine_select(out=ident, in_=onesf, pattern=[[-1, 128]], compare_op=ALU.is_equal,
                                           fill=0.0, base=0, channel_multiplier=1), ["onesf"], ["ident"])
    P.op("pool", lambda e: e.affine_select(out=MU[:, 0:128], in_=onesf, pattern=[[1, 128]], compare_op=ALU.is_gt,
                                           fill=0.0, base=0, channel_multiplier=-1), ["onesf"], ["MU"])
    P.op("pool", lambda e: e.affine_select(out=MU[:, 128:256], in_=onesf, pattern=[[1, 128]], compare_op=ALU.is_ge,
                                           fill=0.0, base=0, channel_multiplier=-1), ["onesf"], ["MU"])
    P.op("pool", lambda e: e.tensor_copy(out=MU[:, 256:512], in_=MU[:, 0:256]), ["MU"], ["MU"])
    P.op("pool", lambda e: e.affine_select(out=SL, in_=onesf, pattern=[[-1, 128]], compare_op=ALU.is_gt,
                                           fill=0.0, base=0, channel_multiplier=1), ["onesf"], ["SL"])
    DMA("sp", pp, pp_d, [], ["pp"])
    DMA("sp", bc, bc_d, [], ["bc"])
    DMA("sp", rkf, rk_d, [], ["rkf"])
    DMA("sp", pos0[:, 0:1], pos0_d, [], ["pos0"])
    CP("pool", RK, rkf, ["rkf"], ["RK"])
    TS("pool", omk, pp[:, PP_KA:PP_KA + 6], -1.0, 1.0, ALU.mult, ALU.add, ["pp"], ["omk"])
    DMA("pool", poolbd, poolbd_d.rearrange("(q p) d -> p q d", p=128), [], ["poolbd"])
    DMA("pool", loraup, loraup_d, [], ["loraup"])
    DMA("pool", gup, gup_d, [], ["gup"])
    w_in_v = w_in_d.rearrange("(k p) n -> p k n", p=128)
    for kc in range(8):
        for hh in range(2):
            DMA("pool", w_in[:, kc, hh * 1408:(hh + 1) * 1408], w_in_v[:, kc, hh * 1408:(hh + 1) * 1408], [], ["w_in"])
    w_out_v = w_out_d.rearrange("(k p) n -> p k n", p=128)
    for kc in range(8):
        DMA("pool", w_out[:, kc, :], w_out_v[:, kc, :], [], ["w_out"])

    xs = [A.bf(NCH * D).rearrange("p (n d) -> p n d", n=NCH) for _ in range(2)]
    xT = A.bf(8 * TT).rearrange("p (k t) -> p k t", k=8)
    lor = A.bf(TT)
    sgb = A.bf(TT)
    zs = [A.f32(TT + 2) for _ in range(2)]
    dtmp = [A.f32(TT) for _ in range(2)]
    zml = A.f32(TT)
    NSET = 2
    km = [A.f32(TT) for _ in range(NSET)]
    rm = [A.f32(TT) for _ in range(NSET)]
    vm = [A.bf(TT) for _ in range(NSET)]
    Tm1 = [A.f32(TT) for _ in range(8)]
    Tm = [Tm1 for _ in range(NSET)]
    sq1 = A.bf(TT)
    sq = [sq1 for _ in range(NSET)]
    AR3 = [A.bf(NCH * 256).rearrange("p (c n) -> p c n", c=NCH) for _ in range(NSET)]
    AR = [a.rearrange("p c (a t) -> p c a t", a=2) for a in AR3]
    bbar = [A.bf(TT) for _ in range(NSET)]
    kbar = [A.bf(TT) for _ in range(NSET)]
    rkb = [A.bf(TT) for _ in range(NSET)]
    tok = [[A.bf(384) for _ in range(NCH)] for _ in range(NSET)]
    gam = [A.f32(NCH) for _ in range(NSET)]
    Ub = [[A.bf(128) for _ in range(NCH)] for _ in range(NSET)]
    NSLOT = NSET * NCH * 2
    M12 = [A.bf(512) for _ in range(NSLOT)]
    M3 = [A.bf(128) for _ in range(NSLOT)]
    ZPP = [[A.bf(384) for _ in range(2)] for _ in range(NSLOT)]
    ZS = [A.bf(128) for _ in range(NSLOT)]
    Zf = [A.bf(128) for _ in range(NSLOT)]
    W1p = [[A.bf(128) for _ in range(NCH)] for _ in range(NSET)]
    catT = A.bf(8 * TT).rearrange("p (k t) -> p k t", k=8)
    Yb = [A.f32(128) for _ in range(2)]
    Ycat = [A.bf(128) for _ in range(2)]
    stat = [A.f32(16) for _ in range(2)]
    pbuf = [A.f32(16 + TT) for _ in range(2)]
    swin2 = [A.f32(16 + TT) for _ in range(2)]
    swin = [swin2[0], swin2[1], swin2[0], swin2[1]]
    invc = [A.f32(TT) for _ in range(2)]
    posf = A.f32(TT)
    diffT = [A.bf(TT) for _ in range(2)]
    mtmp = A.f32(TT)
    xres = [A.f32(D) for _ in range(1)]
    lnst = [A.f32(24) for _ in range(1)]
    print("mixer arena peak", A.peak, A.peak * 4 / 1024, "KB")

    banks = [ps[:, b * 512:(b + 1) * 512] for b in range(8)]
    bctr = [0]
    ctr = {"zs": 0, "py": 0}

    def newbank():
        b = bctr[0]
        bctr[0] = (b + 1) % 8
        return banks[b], ("B", b)

    def nxt(kind, n):
        i = ctr[kind]
        ctr[kind] = (i + 1) % n
        return i

    def TRm(out, in_, r, bk):
        MM(out, in_, ident, True, True, list(r) + ["ident"], [bk])

    for p_ in range(2):
        P.op("pool", lambda e, b=pbuf[p_]: e.memset(b, 0.0), [], [("pbuf", p_)])

    DMA("pool", xs[start_tile % 2], x_ext[start_tile * TT:(start_tile + 1) * TT, :].rearrange("(n p) d -> p n d", p=128),
        [], [("xs", start_tile % 2)])

    def inproj(col):
        bk_ap, bk = newbank()
        out = bk_ap[:, 0:TT]
        for kc in range(8):
            MM(out, w_in[:, kc, col:col + 128], xT[:, kc, :], kc == 0, kc == 7, ["w_in", ("xT", kc // 2)], [bk])
        return out, bk

    def shiftmix(zp, zkey, gi, out, okey):
        zi = nxt("zs", 2)
        z = zs[zi]
        ACT(z[:, 1:TT + 1], zp, AF.Copy, [], [("zsb", zi), zkey])
        CP("pool", z[:, 0:1], carry[:, gi:gi + 1], [("carry", gi)], [("zsc", zi)])
        CP("pool", carry[:, gi:gi + 1], z[:, TT:TT + 1], [("zsb", zi)], [("carry", gi)])
        TTo("dve", dtmp[zi], z[:, 0:TT], zp, ALU.subtract, [("zsb", zi), ("zsc", zi)], [("dtmp", zi), zkey])
        STT(out, dtmp[zi], pp[:, PP_MU + gi:PP_MU + gi + 1], zp, ALU.mult, ALU.add, [("dtmp", zi), "pp"], [okey, zkey])

    import os
    evq = [0]

    def evac_eng():
        evq[0] += 1
        return "act"

    try:
        for tt in range(start_tile, NTILE):
            own = tt >= OWN0
            poolt = tt >= OWN0 - 1
            xsl = tt % 2
            chk("setup", [(ident, "ident"), (MU, "MU"), (SL, "SL"), (bones, "bones"), (pp, "pp")])
            if tt + 1 < NTILE:
                DMA("pool", xs[(tt + 1) % 2],
                    x_ext[(tt + 1) * TT:(tt + 2) * TT, :].rearrange("(n p) d -> p n d", p=128), [], [("xs", (tt + 1) % 2)])
            for j in range(4):
                bk_ap, bk = newbank()
                for kk in range(2):
                    kc = 2 * j + kk
                    for blk in range(NCH):
                        TRm(bk_ap[:, kk * 256 + blk * 128: kk * 256 + (blk + 1) * 128],
                            xs[xsl][:, blk, kc * 128:(kc + 1) * 128], [("xs", xsl)], bk)
                CP(evac_eng(), xT[:, 2 * j:2 * j + 2, :], bk_ap.rearrange("p (a t) -> p a t", a=2), [], [("xT", j), bk])
            chk("xT", [(xT[:, 0, :], ("xT", 0)), (xT[:, 7, :], ("xT", 3))])
            zp, zk = inproj(2560)
            shiftmix(zp, zk, 18, zml, "zml")
            ACT(zml[0:64, :], zml[0:64, :], AF.Sigmoid, ["zml"], ["zml"], scale=2.0)
            TS("dve", lor[0:64, :], zml[0:64, :], 2.0, -1.0, ALU.mult, ALU.add, ["zml"], ["lor"])
            CP("pool", lor[64:128, :], zml[64:128, :], ["zml"], ["lor"])
            if own:
                zp, zk = inproj(2688)
                shiftmix(zp, zk, 19, zml, "zml")
                ACT(sgb, zml, AF.Sigmoid, ["zml"], ["sgb"])
            chk("lor", [(zml, "zml"), (lor, "lor"), (sgb, "sgb")])
            if poolt:
                for q in range(2):
                    zp, zk = inproj(128 * q)
                    pb = pbuf[q]
                    ACT(pb[:, 16:16 + TT], zp, AF.Copy, [], [("pbuf", q), zk])
                    if own:
                        n = 16 + TT
                        s2, s4, s8, s16 = swin
                        TTo("pool", s2[:, 1:n], pb[:, 1:n], pb[:, 0:n - 1], ALU.add, [("pbuf", q)], ["s2"])
                        TTo("pool", s4[:, 3:n], s2[:, 3:n], s2[:, 1:n - 2], ALU.add, ["s2"], ["s4"])
                        if q == 1:
                            TTo("pool", s8[:, 7:n], s4[:, 7:n], s4[:, 3:n - 4], ALU.add, ["s4"], ["s2"])
                            TTo("pool", s16[:, 15:n], s8[:, 15:n], s8[:, 7:n - 8], ALU.add, ["s2"], ["s4"])
                        wins = (2.0, 4.0) if q == 0 else (8.0, 16.0)
                        srcs = (s2, s4) if q == 0 else (s8, s16)
                        skeys = ("s2", "s4")
                        base = (tt - OWN0) * TT + 1
                        P.op("pool", lambda e, b=base: e.iota(posf, pattern=[[1, TT]], base=b, channel_multiplier=0,
                                                                allow_small_or_imprecise_dtypes=True), [], ["posf"])
                        for hf in range(2):
                            rows = slice(64 * hf, 64 * hf + 64)
                            TS("dve", invc[q][rows, :], posf[rows, :], pos0[rows, 0:1], wins[hf], ALU.add, ALU.min,
                               ["posf", "pos0"], [("invc", q)])
                        P.op("dve", lambda e, o=invc[q]: e.reciprocal(out=o, in_=o), [("invc", q)], [("invc", q)])
                        for hf in range(2):
                            rows = slice(64 * hf, 64 * hf + 64)
                            TTo("pool", mtmp[rows, :], srcs[hf][rows, 16:16 + TT], invc[q][rows, :], ALU.mult,
                                [skeys[hf], ("invc", q)], ["mtmp"])
                        TTo("pool", diffT[q], mtmp, pb[:, 16:16 + TT], ALU.subtract, ["mtmp", ("pbuf", q)], [("diffT", q)])
                        bk_ap, bk = newbank()
                        MM(bk_ap[:, 0:TT], poolbd[:, q, :], diffT[q], True, True, ["poolbd", ("diffT", q)], [bk])
                        ACT(catT[:, q, :], bk_ap[:, 0:TT], AF.Copy, ["pp"], [("catT", q), bk],
                            scale=pp[:, PP_PS + q:PP_PS + q + 1])
                    CP("pool", pb[:, 0:16], pb[:, TT:TT + 16], [], [("pbuf", q)])
            def bulk(p):
                st = p % NSET
                T = Tm[st]
                zp, zk = inproj(1024 + 128 * p)
                shiftmix(zp, zk, 6 + p, km[st], ("km", st))
                zp, zk = inproj(1792 + 128 * p)
                shiftmix(zp, zk, 12 + p, vm[st], ("vm", st))
                if own:
                    zp, zk = inproj(256 + 128 * p)
                    shiftmix(zp, zk, p, rm[st], ("rm", st))
                bk_ap, bk = newbank()
                MM(bk_ap[:, 0:TT], loraup[0:64, 128 * p:128 * p + 128], lor[0:64, :], True, True, ["loraup", "lor"], [bk])
                ACT(T[0], bk_ap[:, 0:TT], AF.Sigmoid, ["pp"], [("T0",), bk], bias=pp[:, PP_W0 + p:PP_W0 + p + 1])
                bk_ap, bk = newbank()
                MM(bk_ap[:, 0:TT], loraup[64:128, 128 * p:128 * p + 128], lor[64:128, :], True, True, ["loraup", "lor"], [bk])
                ACT(T[1], bk_ap[:, 0:TT], AF.Sigmoid, ["pp"], [("T1",), bk], bias=pp[:, PP_A0 + p:PP_A0 + p + 1])
                TS("pool", T[2], km[st], pp[:, PP_KK + p:PP_KK + p + 1], None, ALU.mult, None, [("km", st), "pp"], [("T2",)])
                TTo("pool", sq[st], T[2], T[2], ALU.mult, [("T2",)], [("sq",)])
                bk_ap, bk = newbank()
                MM(bk_ap[:, 0:TT], bones, sq[st], True, True, ["bones", ("sq",)], [bk])
                TS("dve", T[3], bk_ap[:, 0:TT], 1e-24, None, ALU.max, None, [], [("T3",), bk])
                ACT(T[3], T[3], AF.Ln, [("T3",)], [("T3",)])
                ACT(T[3], T[3], AF.Exp, [("T3",)], [("T3",)], scale=-0.5)
                TTo("pool", T[2], T[2], T[3], ALU.mult, [("T2",), ("T3",)], [("T2",)])
                TTo("pool", T[4], T[2], T[1], ALU.mult, [("T2",), ("T1",)], [("T4",)])
                TS("dve", T[5], T[1], pp[:, PP_KA + p:PP_KA + p + 1], omk[:, p:p + 1], ALU.mult, ALU.add,
                   [("T1",), "pp", "omk"], [("T5",)])
                TTo("pool", T[5], km[st], T[5], ALU.mult, [("km", st), ("T5",)], [("T5",)])
                for c in range(NCH):
                    cs = slice(c * 128, (c + 1) * 128)
                    P.op("dve", lambda e, o=T[6][:, cs], i=T[0][:, cs]: e.tensor_tensor_scan(
                        out=o, data0=i, data1=zeros, initial=0.0, op0=ALU.add, op1=ALU.add),
                        [("T0",), "zeros"], [("T6",)])
                ACT(T[7], T[6], AF.Exp, [("T6",)], [("T7",)], scale=SC)
                ACT(T[3], T[6], AF.Exp, [("T6",), ("T2",)], [("T3",)], scale=-SC)
                T2v = T[2].rearrange("p (c t) -> p c t", c=NCH)
                T7v = T[7].rearrange("p (c t) -> p c t", c=NCH)
                ark = ("AR", st)
                STT(AR[st][:, :, 0, 1:128], T2v[:, :, 1:128], -1.0, T7v[:, :, 0:127], ALU.mult, ALU.mult,
                    [("T2",), ("T7",)], [ark])
                TS("pool", AR[st][:, :, 0, 0:1], T2v[:, :, 0:1], -1.0, None, ALU.mult, None, [("T2",)], [ark])
                TTo("pool", bbar[st], T[4], T[3], ALU.mult, [("T4",), ("T3",)], [("bbar", st)])
                TTo("dve", kbar[st], T[5], T[3], ALU.mult, [("T5",), ("T3",)], [("kbar", st)])
                CP("pool", gam[st].rearrange("p (c o) -> p c o", o=1), T7v[:, :, 127:128], [("T7",)], [("gam", st)])
                if own:
                    TTo("pool", AR[st][:, :, 1, :], rm[st].rearrange("p (c t) -> p c t", c=NCH), T7v, ALU.mult,
                        [("rm", st), ("T7",)], [ark])
                    TTo("pool", rkb[st], rm[st], T[5], ALU.mult, [("rm", st), ("T5",)], [("rkb", st)])
                for c in range(NCH):
                    cs = slice(c * 128, (c + 1) * 128)
                    bk_ap, bk = newbank()
                    for j, (src, skey) in enumerate(((bbar[st], ("bbar", st)), (kbar[st], ("kbar", st)), (vm[st], ("vm", st)))):
                        TRm(bk_ap[:, j * 128:(j + 1) * 128], src[:, cs], [skey], bk)
                    CP(evac_eng(), tok[st][c], bk_ap[:, 0:384], [], [("tok", st, c), bk])

            def units(p):
                st = p % NSET
                return [(c, e2, (st * NCH + c) * 2 + e2) for c in range(NCH) for e2 in range(2)]

            def abuild(p):
                st = p % NSET
                ark = ("AR", st)
                wA = 256 if own else 128
                for c in range(NCH):
                    cs = slice(c * 128, (c + 1) * 128)
                    bks = [newbank() for _ in range(2)]
                    for (src, skey, off) in ((bbar[st], ("bbar", st), 0), (kbar[st], ("kbar", st), 256)):
                        for e2 in range(2):
                            rows = slice(64 * e2, 64 * e2 + 64)
                            MM(bks[e2][0][:, off:off + wA], src[rows, cs], AR3[st][rows, c, 0:wA], True, True,
                               [skey, ark], [bks[e2][1]])
                    for e2 in range(2):
                        s = (st * NCH + c) * 2 + e2
                        bk_ap, bk = bks[e2]
                        if own:
                            TTo("dve", M12[s], bk_ap, MU, ALU.mult, ["MU"], [("M12", s), bk])
                        else:
                            v3 = lambda a: a.rearrange("p (a b) -> p a b", a=2)[:, :, 0:128]
                            TTo("dve", v3(M12[s]), v3(bk_ap), v3(MU), ALU.mult, ["MU"], [("M12", s), bk])
                for (c, e2, s) in units(p):
                    cs = slice(c * 128, (c + 1) * 128)
                    rows = slice(64 * e2, 64 * e2 + 64)
                    bk_ap, bk = newbank()
                    MM(bk_ap[:, 0:128], AR3[st][rows, c, 0:128], bbar[st][rows, cs], True, True, [ark, ("bbar", st)], [bk])
                    TTo("dve", M3[s], bk_ap[:, 0:128], SL, ALU.mult, ["SL"], [("M3", s), bk])

            curz = {}

            def chain_iter(p, it):
                for (c, e2, s) in units(p):
                    zk_ = lambda i: ("ZPP", s, i)
                    if it == 0:
                        Nn = M12[s][:, 0:128]
                        bk_ap, bk = newbank()
                        MM(bk_ap[:, 0:128], M3[s], Nn, True, True, [("M3", s), ("M12", s)], [bk])
                        MM(bk_ap[:, 128:256], Nn, M3[s], True, True, [("M3", s), ("M12", s)], [bk])
                        CP("pool", ZPP[s][0][:, 0:128], Nn, [("M12", s)], [zk_(0)])
                        ACT(ZPP[s][0][:, 128:384], bk_ap[:, 0:256], AF.Copy, [], [zk_(0), bk])
                        curz[s] = 0
                        continue
                    cur = curz[s]
                    nx = 1 - cur
                    Z = ZPP[s][cur]
                    TTo("pool", ZS[s], Z[:, 0:128], Z[:, 128:256], ALU.add, [zk_(cur)], [("ZS", s)])
                    bk_ap, bk = newbank()
                    if it < 6:
                        MM(bk_ap[:, 0:256], Z[:, 256:384], Z[:, 0:256], True, True, [zk_(cur)], [bk])
                        MM(bk_ap[:, 256:384], Z[:, 128:256], Z[:, 256:384], True, True, [zk_(cur)], [bk])
                        TTo("dve", ZPP[s][nx][:, 0:128], bk_ap[:, 0:128], ZS[s], ALU.add, [("ZS", s)], [zk_(nx), bk])
                        ACT(ZPP[s][nx][:, 128:384], bk_ap[:, 128:384], AF.Copy, [], [zk_(nx), bk])
                    else:
                        MM(bk_ap[:, 0:128], Z[:, 256:384], Z[:, 0:128], True, True, [zk_(cur)], [bk])
                        TTo("dve", Zf[s], bk_ap[:, 0:128], ZS[s], ALU.add, [("ZS", s)], [("Zf", s), bk])
                    curz[s] = nx

            def seq_step(p, k):
                if k >= 3 * NCH:
                    return
                st = p % NSET
                ark = ("AR", st)
                c = k // 3
                step = k % 3
                cs = slice(c * 128, (c + 1) * 128)
                tkk = ("tok", st, c)
                hk = ("Hb", p)
                uk = ("U", st, c)
                wk = ("W1p", st, c)
                sl_ = [(st * NCH + c) * 2 + e2 for e2 in range(2)]
                if step == 0:
                    bk_ap, bk = newbank()
                    for e2 in range(2):
                        rows = slice(64 * e2, 64 * e2 + 64)
                        s = sl_[e2]
                        vt = tok[st][c][:, 256 + 64 * e2:256 + 64 * e2 + 64]
                        oo = bk_ap[:, 64 * e2:64 * e2 + 64]
                        MM(oo, AR3[st][rows, c, 0:128], Hb[rows, p, :], True, False, [ark, hk], [bk])
                        MM(oo, M12[s][:, 256:384], vt, False, True, [("M12", s), tkk], [bk])
                    ACT(W1p[st][c], bk_ap[:, 0:128], AF.Copy, [], [wk, bk])
                    return
                if step == 1:
                    bk_ap, bk = newbank()
                    for e2 in range(2):
                        s = sl_[e2]
                        MM(bk_ap[:, 64 * e2:64 * e2 + 64], Zf[s], W1p[st][c][:, 64 * e2:64 * e2 + 64], True, True,
                           [("Zf", s), wk], [bk])
                    TTo("dve", Ub[st][c], bk_ap[:, 0:128], W1p[st][c], ALU.add, [wk], [uk, bk])
                    return
                bk_ap, bk = newbank()
                if own:
                    for e2 in range(2):
                        rows = slice(64 * e2, 64 * e2 + 64)
                        s = sl_[e2]
                        vt = tok[st][c][:, 256 + 64 * e2:256 + 64 * e2 + 64]
                        oo = bk_ap[:, 64 * e2:64 * e2 + 64]
                        MM(oo, AR3[st][rows, c, 128:256], Hb[rows, p, :], True, False, [ark, hk], [bk])
                        MM(oo, M12[s][:, 128:256], Ub[st][c][:, 64 * e2:64 * e2 + 64], False, False, [("M12", s), uk], [bk])
                        MM(oo, M12[s][:, 384:512], vt, False, True, [("M12", s), tkk], [bk])
                hp = bk_ap[:, 128:256]
                MM(hp, tok[st][c][:, 0:128], Ub[st][c], True, False, [tkk, uk], [bk])
                MM(hp, tok[st][c][:, 128:256], tok[st][c][:, 256:384], False, True, [tkk], [bk])
                if own:
                    MM(bk_ap[:, 256:258], rkb[st][:, cs], RK[:, 2 * p:2 * p + 2], True, True, [("rkb", st), "RK"], [bk])
                    MM(bk_ap[:, 384:512], sgb[:, cs], gup[:, 128 * p:128 * p + 128], True, True, ["sgb", "gup"], [bk])
                TTo("dve", tmpH[0:64, :], hp[0:64, 0:64], Hf[0:64, p, :], ALU.add, [("Hf", p)], ["tmpH", bk])
                TTo("dve", tmpH[64:128, :], hp[64:128, 64:128], Hf[64:128, p, :], ALU.add, [("Hf", p)], ["tmpH", bk])
                ACT(Hf[:, p, :], tmpH, AF.Copy, ["tmpH", ("gam", st)], [("Hf", p)], scale=gam[st][:, c:c + 1])
                CP("pool", Hb[:, p, :], Hf[:, p, :], [("Hf", p)], [hk])
                if own:
                    yi = nxt("py", 2)
                    Y = Yb[yi]
                    yk = ("Y", yi)
                    sti = stat[yi]
                    sk_ = ("stat", yi)
                    for e2 in range(2):
                        P.op("dve", lambda e, o=sti[:, 6 * e2:6 * e2 + 6], i=bk_ap[:, 64 * e2:64 * e2 + 64]:
                             e.bn_stats(out=o, in_=i), [], [sk_, bk])
                        P.op("dve", lambda e, o=sti[:, 12 + 2 * e2:14 + 2 * e2], i=sti[:, 6 * e2:6 * e2 + 6]:
                             e.bn_aggr(out=o, in_=i), [sk_], [sk_])
                    for e2 in range(2):
                        ACT(sti[:, 13 + 2 * e2:14 + 2 * e2], sti[:, 13 + 2 * e2:14 + 2 * e2], AF.Ln,
                            [sk_, "epsx"], [sk_], bias=epsx[:, 0:1])
                        ACT(sti[:, 13 + 2 * e2:14 + 2 * e2], sti[:, 13 + 2 * e2:14 + 2 * e2], AF.Exp,
                            [sk_], [sk_], scale=-0.5)
                    for e2 in range(2):
                        TS("dve", Y[:, 64 * e2:64 * e2 + 64], bk_ap[:, 64 * e2:64 * e2 + 64],
                           sti[:, 12 + 2 * e2:13 + 2 * e2], sti[:, 13 + 2 * e2:14 + 2 * e2], ALU.subtract, ALU.mult,
                           [sk_], [yk, bk])
                    TTo("pool", Y, Y, bc[:, BC_LNXG + 128 * p:BC_LNXG + 128 * p + 128], ALU.mult, [yk, "bc"], [yk])
                    TTo("pool", Y, Y, bc[:, BC_LNXB + 128 * p:BC_LNXB + 128 * p + 128], ALU.add, [yk, "bc"], [yk])
                    for e2 in range(2):
                        STT(Y[:, 64 * e2:64 * e2 + 64], tok[st][c][:, 256 + 64 * e2:256 + 64 * e2 + 64],
                            bk_ap[:, 256 + e2:257 + e2], Y[:, 64 * e2:64 * e2 + 64], ALU.mult, ALU.add,
                            [tkk, yk], [yk, bk])
                    TTo("dve", Ycat[yi], Y, bk_ap[:, 384:512], ALU.mult, [yk], [("Ycat", yi), bk])
                    bk2_ap, bk2 = newbank()
                    TRm(bk2_ap[:, 0:128], Ycat[yi], [("Ycat", yi)], bk2)
                    ACT(catT[:, 2 + p, cs], bk2_ap[:, 0:128], AF.Copy, [], [("catT", 2 + p), bk2])

            for p in range(7):
                if p < 6:
                    bulk(p)
                    abuild(p)
                for it in range(7):
                    if p < 6:
                        chain_iter(p, it)
                    if p >= 1:
                        seq_step(p - 1, it)
            chk("seq", [(Hf.rearrange("p a b -> p (a b)"), ("Hf", 5)), (catT[:, 0, :], ("catT", 0)), (catT[:, 1, :], ("catT", 1)),
                        (catT[:, 2, :], ("catT", 2)), (catT[:, 7, :], ("catT", 7))])
            if own:
                for blk in range(NCH):
                    cs = slice(blk * 128, (blk + 1) * 128)
                    xr = xres[0]
                    xk = ("xres", 0)
                    row0 = tt * TT + blk * 128
                    DMA("sp", xr, x_ext[row0:row0 + 128, :], [], [xk])
                    for dh in range(2):
                        bk_ap, bk = newbank()
                        for kc in range(8):
                            MM(bk_ap, catT[:, kc, cs], w_out[:, kc, dh * 512:(dh + 1) * 512], kc == 0, kc == 7,
                               [("catT", kc), "w_out"], [bk])
                        STT(xr[:, dh * 512:(dh + 1) * 512], xr[:, dh * 512:(dh + 1) * 512], ALPHA, bk_ap, ALU.mult, ALU.add,
                            [], [xk, bk])
                    ls = lnst[0]
                    lk = ("lnst", 0)
                    for dh in range(2):
                        P.op("dve", lambda e, o=ls[:, 6 * dh:6 * dh + 6], i=xr[:, dh * 512:(dh + 1) * 512]:
                             e.bn_stats(out=o, in_=i), [xk], [lk])
                    P.op("dve", lambda e, o=ls[:, 12:14], i=ls[:, 0:12]: e.bn_aggr(out=o, in_=i), [lk], [lk])
                    ACT(ls[:, 13:14], ls[:, 13:14], AF.Ln, [lk, "eps1"], [lk], bias=eps1[:, 0:1])
                    ACT(ls[:, 13:14], ls[:, 13:14], AF.Exp, [lk], [lk], scale=-0.5)
                    TS("dve", xr, xr, ls[:, 12:13], ls[:, 13:14], ALU.subtract, ALU.mult, [xk, lk], [xk])
                    TTo("pool", xr, xr, bc[:, BC_LN1G:BC_LN1G + D], ALU.mult, [xk, "bc"], [xk])
                    TTo("pool", xr, xr, bc[:, BC_LN1B:BC_LN1B + D], ALU.add, [xk, "bc"], [xk])
                    r0 = (tt - OWN0) * TT + blk * 128
                    hdst = out_d if (debug_h or not do_moe) else hbuf
                    DMA("sp", hdst[r0:r0 + 128, :], xr, [xk], [("hbuf", r0 // 128)])
    except StopBuild:
        pass

    if do_moe and not debug_h:
        build_moe(nc, P, A, ps, hbuf, out_d, bc2_d, router_d, eg_d, eu_d, ed_d,
                  (TTo, TS, STT, ACT, CP, MM, DMA, newbank, ident))

    fin = [(s, v) for s, v in P.dma_val.items()]
    waits = P._waits("sp", fin)
    if waits:
        P.q["sp"].append((waits, None, None))

    nc._prog_log = P.log
    names = P.sem_names()
    semh = {}
    cms = []
    for n in names:
        cm = nc.semaphore(n)
        cms.append(cm)
        semh[n] = cm.__enter__()
    P.emit(semh)
    for cm in reversed(cms):
        cm.__exit__(None, None, None)
    psum_t.__exit__(None, None, None)
    arena_t.__exit__(None, None, None)
    return nc


def build_moe(nc, P, A, ps, hbuf, out_d, bc2_d, router_d, eg_d, eu_d, ed_d, H):
    (TTo, TS, STT, ACT, CP, MM, DMA, newbank, ident) = H
    P.barrier()
    A.off = 0
    NT_ = OWN // 128
    identm = A.bf(128)
    onesm = A.f32(128)
    bc2 = A.f32(BC2_N)
    eps2 = A.f32(2)
    routf = A.f32(8 * 36)
    rout = A.bf(8 * 36).rearrange("p (k n) -> p k n", k=8)
    yacc = A.f32(NT_ * D).rearrange("p (i d) -> p i d", i=NT_)
    hT = A.bf(8 * OWN).rearrange("p (k t) -> p k t", k=8)
    Wt = A.f32(NT_ * 32).rearrange("p (i e) -> p i e", i=NT_)
    hld = [A.f32(D) for _ in range(2)]
    hbf = [A.bf(D) for _ in range(2)]
    Wg = [A.bf(8 * 256).rearrange("p (k n) -> p k n", k=8) for _ in range(2)]
    Wu = [A.bf(8 * 256).rearrange("p (k n) -> p k n", k=8) for _ in range(2)]
    Wd = [A.bf(2 * D).rearrange("p (k n) -> p k n", k=2) for _ in range(2)]
    hid = [A.bf(2 * OWN).rearrange("p (k t) -> p k t", k=2) for _ in range(2)]
    sgt = [A.bf(512) for _ in range(2)]
    rt = A.f32(256)
    lnst = A.f32(24)
    print("moe arena peak", A.off, A.off * 4 / 1024, "KB")

    P.op("pool", lambda e: e.memset(onesm, 1.0), [], ["onesm"])
    P.op("pool", lambda e: e.affine_select(out=identm, in_=onesm, pattern=[[-1, 128]], compare_op=ALU.is_equal,
                                           fill=0.0, base=0, channel_multiplier=1), ["onesm"], ["identm"])
    P.op("pool", lambda e: e.memset(eps2, LN_EPS), [], ["eps2"])
    DMA("sp", bc2, bc2_d, [], ["bc2"])
    DMA("sp", routf.rearrange("p (k n) -> p k n", k=8), router_d.rearrange("(k p) n -> p k n", p=128), [], ["routf"])
    CP("pool", rout.rearrange("p k n -> p (k n)"), routf, ["routf"], ["rout"])

    def load_w(e):
        b = e % 2
        DMA("pool", Wg[b], eg_d[e].rearrange("(k p) n -> p k n", p=128), [], [("Wg", b)])
        DMA("pool", Wu[b], eu_d[e].rearrange("(k p) n -> p k n", p=128), [], [("Wu", b)])
        DMA("pool", Wd[b], ed_d[e].rearrange("(k p) n -> p k n", p=128), [], [("Wd", b)])

    load_w(0)
    for i in range(NT_):
        hb = i % 2
        hk = ("hld", hb)
        DMA("sp", hld[hb], hbuf[i * 128:(i + 1) * 128, :], [("hbuf", i)], [hk])
        ACT(yacc[:, i, :], hld[hb], AF.Copy, [hk], [("yacc", i)], scale=ALPHA)
        CP("pool", hbf[hb], hld[hb], [hk], [("hbf", hb)])
        for j in range(2):
            bk_ap, bk = newbank()
            for kk in range(4):
                kc = 4 * j + kk
                MM(bk_ap[:, kk * 128:(kk + 1) * 128], hbf[hb][:, kc * 128:(kc + 1) * 128], identm, True, True,
                   [("hbf", hb), "identm"], [bk])
            CP("act" if j == 0 else "dve", hT[:, 4 * j:4 * j + 4, i * 128:(i + 1) * 128],
               bk_ap.rearrange("p (a t) -> p a t", a=4), [], [("hT", i), bk])
        bk_ap, bk = newbank()
        for kc in range(8):
            MM(bk_ap[:, 0:36], hT[:, kc, i * 128:(i + 1) * 128], rout[:, kc, :], kc == 0, kc == 7, [("hT", i), "rout"], [bk])
        lg = rt[:, 0:36]
        rk_ = "rt"
        TTo("dve", lg, bk_ap[:, 0:36], bc2[:, BC2_RB:BC2_RB + 36], ALU.add, ["bc2"], [rk_, bk])
        gl = rt[:, 0:4]
        gmax = rt[:, 40:41]
        ngmax = rt[:, 41:42]
        gsum = rt[:, 42:43]
        gexp = rt[:, 44:48]
        goh = rt[:, 48:52]
        esel = rt[:, 56:64]
        m8 = rt[:, 64:72]
        w1 = rt[:, 72:73]
        w2 = rt[:, 73:74]
        tmp8 = rt[:, 80:88]
        we8 = rt[:, 88:96]
        gsc = rt[:, 96:100]
        P.op("dve", lambda e: e.tensor_reduce(out=gmax, in_=gl, axis=mybir.AxisListType.X, op=ALU.max), [rk_], [rk_])
        TS("dve", ngmax, gmax, -1.0, None, ALU.mult, None, [rk_], [rk_])
        ACT(gexp, gl, AF.Exp, [rk_], [rk_], bias=ngmax)
        P.op("dve", lambda e: e.tensor_reduce(out=gsum, in_=gexp, axis=mybir.AxisListType.X, op=ALU.add), [rk_], [rk_])
        P.op("dve", lambda e: e.reciprocal(out=gsum, in_=gsum), [rk_], [rk_])
        TS("dve", goh, gl, gmax, None, ALU.is_equal, None, [rk_], [rk_])
        TS("dve", esel, rt[:, 4:12], goh[:, 0:1], None, ALU.mult, None, [rk_], [rk_])
        for g in range(1, 4):
            STT(esel, rt[:, 4 + 8 * g:12 + 8 * g], goh[:, g:g + 1], esel, ALU.mult, ALU.add, [rk_], [rk_])
        P.op("dve", lambda e: e.max(out=m8, in_=esel), [rk_], [rk_])
        TTo("dve", w2, m8[:, 1:2], m8[:, 0:1], ALU.subtract, [rk_], [rk_])
        ACT(w2, w2, AF.Exp, [rk_], [rk_])
        TS("dve", w1, w2, 1.0, None, ALU.add, None, [rk_], [rk_])
        P.op("dve", lambda e: e.reciprocal(out=w1, in_=w1), [rk_], [rk_])
        TTo("dve", w2, w2, w1, ALU.mult, [rk_], [rk_])
        TS("dve", we8, esel, m8[:, 0:1], w1, ALU.is_equal, ALU.mult, [rk_], [rk_])
        TS("dve", tmp8, esel, m8[:, 1:2], w2, ALU.is_equal, ALU.mult, [rk_], [rk_])
        TTo("dve", we8, we8, tmp8, ALU.add, [rk_], [rk_])
        TS("dve", gsc, goh, gsum, None, ALU.mult, None, [rk_], [rk_])
        for g in range(4):
            TS("dve", Wt[:, i, 8 * g:8 * g + 8], we8, gsc[:, g:g + 1], None, ALU.mult, None, [rk_], [("Wt", i)])

    for e in range(32):
        b = e % 2
        if e + 1 < 32:
            load_w(e + 1)
        hd = hid[b]
        for tb in range(4):
            ts_ = slice(tb * 512, (tb + 1) * 512)
            for half in range(2):
                g_ap, gk = newbank()
                for kc in range(8):
                    MM(g_ap, Wg[b][:, kc, half * 128:(half + 1) * 128], hT[:, kc, ts_], kc == 0, kc == 7,
                       [("Wg", b)] + [("hT", 4 * tb + q) for q in range(4)], [gk])
                u_ap, uk_ = newbank()
                for kc in range(8):
                    MM(u_ap, Wu[b][:, kc, half * 128:(half + 1) * 128], hT[:, kc, ts_], kc == 0, kc == 7,
                       [("Wu", b)] + [("hT", 4 * tb + q) for q in range(4)], [uk_])
                si = (tb * 2 + half) % 2
                ACT(sgt[si], g_ap, AF.Silu, [], [("sgt", si), gk])
                TTo("dve", hd[:, half, ts_], sgt[si], u_ap, ALU.mult, [("sgt", si)], [("hid", b, tb), uk_])
        for i in range(NT_):
            for dh in range(2):
                y_ap, yk_ = newbank()
                for half in range(2):
                    MM(y_ap, hd[:, half, i * 128:(i + 1) * 128], Wd[b][:, half, dh * 512:(dh + 1) * 512], half == 0, half == 1,
                       [("hid", b, i // 4), ("Wd", b)], [yk_])
                STT(yacc[:, i, dh * 512:(dh + 1) * 512], y_ap, Wt[:, i, e:e + 1], yacc[:, i, dh * 512:(dh + 1) * 512],
                    ALU.mult, ALU.add, [("Wt", i)], [("yacc", i), yk_])
    for i in range(NT_):
        y = yacc[:, i, :]
        yk = ("yacc", i)
        lk = "lnst2"
        for dh in range(2):
            P.op("dve", lambda e, o=lnst[:, 6 * dh:6 * dh + 6], i_=y[:, dh * 512:(dh + 1) * 512]:
                 e.bn_stats(out=o, in_=i_), [yk], [lk])
        P.op("dve", lambda e: e.bn_aggr(out=lnst[:, 12:14], in_=lnst[:, 0:12]), [lk], [lk])
        ACT(lnst[:, 13:14], lnst[:, 13:14], AF.Ln, [lk, "eps2"], [lk], bias=eps2[:, 0:1])
        ACT(lnst[:, 13:14], lnst[:, 13:14], AF.Exp, [lk], [lk], scale=-0.5)
        TS("dve", y, y, lnst[:, 12:13], lnst[:, 13:14], ALU.subtract, ALU.mult, [yk, lk], [yk])
        TTo("pool", y, y, bc2[:, BC2_LN2G:BC2_LN2G + D], ALU.mult, [yk, "bc2"], [yk])
        TTo("pool", y, y, bc2[:, BC2_LN2B:BC2_LN2B + D], ALU.add, [yk, "bc2"], [yk])
        DMA("sp", out_d[i * 128:(i + 1) * 128, :], y, [yk], [("out", i)])


def prep_inputs(inputs):
    f = lambda k: np.ascontiguousarray(np.asarray(inputs[k], dtype=np.float32)[0])
    x = np.asarray(inputs["x"], dtype=np.float32)
    mu = f("mu_shift")
    pp = np.zeros((128, PP_N), np.float32)
    pp[:, PP_MU:PP_MU + 20] = mu.reshape(20, 128).T
    pp[:, PP_W0:PP_W0 + 6] = f("w0").reshape(6, 128).T
    pp[:, PP_A0:PP_A0 + 6] = f("a0").reshape(6, 128).T
    pp[:, PP_KK:PP_KK + 6] = f("k_k").reshape(6, 128).T
    pp[:, PP_KA:PP_KA + 6] = f("k_a").reshape(6, 128).T
    pp[:, PP_PS:PP_PS + 2] = f("pool_scale").reshape(2, 128).T
    bc = np.zeros((128, BC_N), np.float32)
    bc[:, BC_LNXG:BC_LNXG + 768] = f("lnx_g")[None, :]
    bc[:, BC_LNXB:BC_LNXB + 768] = f("lnx_b")[None, :]
    bc[:, BC_LN1G:BC_LN1G + D] = f("ln1_g")[None, :]
    bc[:, BC_LN1B:BC_LN1B + D] = f("ln1_b")[None, :]
    bc2 = np.zeros((128, BC2_N), np.float32)
    bc2[:, BC2_LN2G:BC2_LN2G + D] = f("ln2_g")[None, :]
    bc2[:, BC2_LN2B:BC2_LN2B + D] = f("ln2_b")[None, :]
    bc2[:, BC2_RB:BC2_RB + 4] = f("router_group_b")[None, :]
    bc2[:, BC2_RB + 4:BC2_RB + 36] = f("router_expert_b")[None, :]
    rk = f("r_k")
    rkm = np.zeros((128, 12), np.float32)
    for h in range(12):
        e = h % 2
        rkm[64 * e:64 * e + 64, h] = rk[h]
    pw = f("pool_w")
    poolbd = np.zeros((256, 128), np.float32)
    for g in range(4):
        q, e = g // 2, g % 2
        poolbd[128 * q + 64 * e:128 * q + 64 * e + 64, 64 * e:64 * e + 64] = pw[g]
    loraup = np.concatenate([f("w_up"), f("a_up")], axis=0)
    router = np.concatenate([f("router_group"), f("router_expert")], axis=1)
    shared = {
        "w_in": f("w_in"), "pp": pp, "bc": bc, "bc2": bc2, "rkm": rkm, "poolbd": poolbd,
        "loraup": np.ascontiguousarray(loraup), "gup": f("g_up"), "w_out": f("w_out"),
        "router": np.ascontiguousarray(router), "exp_gate": f("exp_gate"), "exp_up": f("exp_up"),
        "exp_down": f("exp_down"),
    }
    in_maps = []
    for c in range(NCORE):
        b, j = c // 4, c % 4
        xe = np.zeros((SEQ, D), np.float32)
        n = OWN * (j + 1)
        xe[SEQ - n:, :] = x[b, 0:n, :]
        m = dict(shared)
        m["x_ext"] = xe
        m["pos0"] = np.full((128, 1), float(OWN * j), np.float32)
        in_maps.append(m)
    return in_maps


def kernel(**inputs):
    in_maps = prep_inputs(inputs)
    nc = build()
    res = run_bass_kernel_spmd(nc, in_maps, core_ids=list(range(NCORE)))
    out = np.zeros((2, SEQ, D), np.float32)
    for c in range(NCORE):
        b, j = c // 4, c % 4
        out[b, OWN * j:OWN * (j + 1), :] = res.results[c]["out"]
    return out
```
